# Optimizing a Trainium2 kernel written in Bass

```python
import jax, jax.numpy as jnp
from jax import lax
import numpy as np

D_MODEL = 2048
BATCH = 4
SEQ = 4096
DEPTH = 2

GRID_W = 64
CTX_LEN = 256
N_MIXERS = 2
N_GLA_LAYERS = (DEPTH + N_MIXERS - 1) // N_MIXERS
N_NAT_LAYERS = DEPTH // N_MIXERS

GLA_HEADS = 4
GLA_DK = D_MODEL // (2 * GLA_HEADS)
GLA_DV = D_MODEL // GLA_HEADS
GLA_CHUNK = 64
GLA_GATE_RANK = 16
GLA_GATE_NORM = 16.0
GLA_QK = GLA_HEADS * GLA_DK
GLA_VD = GLA_HEADS * GLA_DV
GLA_IN = 2 * GLA_QK + 2 * GLA_VD + 2 * GLA_GATE_RANK
ROPE_BASE = 10000.0

NAT_HEADS = 16
NAT_DH = D_MODEL // NAT_HEADS
WIN_R = 8
WIN_C = 16
NAT_QC = 16

N_GROUPS = 4
EXPERTS_PER_GROUP = 4
N_EXPERTS = N_GROUPS * EXPERTS_PER_GROUP
TOP_K_IN_GROUP = 2
D_EXPERT = D_MODEL // 2
MOE_BLOCK = 256

DEEPNORM_ALPHA = (2 * DEPTH) ** 0.25
DEEPNORM_BETA = (8 * DEPTH) ** -0.25
LN_EPS = 1e-5
NEG_INF = -1e30

kernel_name = "hybrid_gla_natten_hmoe_dit"


def _layer_norm(x, g, b):
    xf = x.astype(jnp.float32)
    mu = jnp.mean(xf, axis=-1, keepdims=True)
    var = jnp.mean(jnp.square(xf - mu), axis=-1, keepdims=True)
    return ((xf - mu) * lax.rsqrt(var + LN_EPS) * g + b).astype(x.dtype)


def _rope_axis(x, pos):
    half = x.shape[-1] // 2
    freqs = ROPE_BASE ** (-jnp.arange(half, dtype=jnp.float32) / half)
    ang = pos.astype(jnp.float32)[:, None] * freqs
    cos, sin = jnp.cos(ang), jnp.sin(ang)
    x1, x2 = x[..., :half], x[..., half:]
    return jnp.concatenate([x1 * cos - x2 * sin, x1 * sin + x2 * cos], axis=-1)


def _rope_2d(x):
    t = jnp.arange(x.shape[-2])
    d_ax = x.shape[-1] // 2
    return jnp.concatenate([_rope_axis(x[..., :d_ax], t // GRID_W),
                            _rope_axis(x[..., d_ax:], t % GRID_W)], axis=-1)


def _gla_chunked(q, k, v, g, s0):
    b_, h_, n, _ = q.shape
    dv = v.shape[-1]
    nc = n // GLA_CHUNK
    ch = lambda t: t.reshape(b_, h_, nc, GLA_CHUNK, t.shape[-1])
    q, k, v, g = ch(q), ch(k), ch(v), ch(g)
    cum = jnp.cumsum(g, axis=3)
    total = cum[:, :, :, -1]
    q_dec = q * jnp.exp(cum)
    k_inv = k * jnp.exp(-cum)
    k_end = k * jnp.exp(total[:, :, :, None] - cum)
    tri = jnp.tril(jnp.ones((GLA_CHUNK, GLA_CHUNK), dtype=bool))
    att = jnp.where(tri, jnp.einsum('bhncd,bhnsd->bhncs', q_dec, k_inv), 0.0)
    o_intra = jnp.einsum('bhncs,bhnsv->bhncv', att, v)

    def step(state, xs):
        q_c, k_c, v_c, d_c = xs
        o_c = jnp.einsum('bhcd,bhdv->bhcv', q_c, state)
        state = state * d_c[..., None] + jnp.einsum('bhcd,bhcv->bhdv', k_c, v_c)
        return state, o_c

    xs = (jnp.moveaxis(q_dec, 2, 0), jnp.moveaxis(k_end, 2, 0), jnp.moveaxis(v, 2, 0),
          jnp.moveaxis(jnp.exp(total), 2, 0))
    s_fin, o_inter = lax.scan(step, s0, xs)
    o = o_intra + jnp.moveaxis(o_inter, 0, 2)
    return o.reshape(b_, h_, n, dv), s_fin


def _gla_mixer(h_lat, h_ctx, w_in, w_gate, b_gate, norm_g, w_out):
    def project(h):
        bsz, n, _ = h.shape
        p = h @ w_in
        q, k, v, r, glr = jnp.split(p, [GLA_QK, 2 * GLA_QK, 2 * GLA_QK + GLA_VD, 2 * GLA_QK + 2 * GLA_VD], axis=-1)
        heads = lambda t, d: t.reshape(bsz, n, GLA_HEADS, d).transpose(0, 2, 1, 3).astype(jnp.float32)
        glr = glr.reshape(bsz, n, 2, GLA_GATE_RANK)
        z = jnp.einsum('bndr,drk->bndk', glr, w_gate) + b_gate
        logdecay = jax.nn.log_sigmoid(z.astype(jnp.float32)) / GLA_GATE_NORM
        return (heads(q, GLA_DK) * GLA_DK ** -0.5, heads(k, GLA_DK), heads(v, GLA_DV), r,
                heads(logdecay[:, :, 0], GLA_DK), heads(logdecay[:, :, 1], GLA_DK))

    ql, kl, vl, rl, gfl, gbl = project(h_lat)
    ql, kl = _rope_2d(ql), _rope_2d(kl)
    qc, kc, vc, rc, gfc, gbc = project(h_ctx)
    bsz = h_lat.shape[0]
    s0 = jnp.zeros((bsz, GLA_HEADS, GLA_DK, GLA_DV), jnp.float32)
    flip = lambda t: jnp.flip(t, axis=2)
    o_cf, s_f = _gla_chunked(qc, kc, vc, gfc, s0)
    o_lf, _ = _gla_chunked(ql, kl, vl, gfl, s_f)
    o_cb, s_b = _gla_chunked(flip(qc), flip(kc), flip(vc), flip(gbc), s0)
    o_lb, _ = _gla_chunked(flip(ql), flip(kl), flip(vl), flip(gbl), s_b)

    def finish(o, r):
        o = o * lax.rsqrt(jnp.mean(jnp.square(o), axis=-1, keepdims=True) + LN_EPS) * norm_g
        b_, _, n, _ = o.shape
        o = o.transpose(0, 2, 1, 3).reshape(b_, n, GLA_VD).astype(r.dtype)
        return (o * jax.nn.silu(r)) @ w_out

    return finish(o_lf + flip(o_lb), rl), finish(o_cf + flip(o_cb), rc)


def _natten_layout(rows):
    kr = min(WIN_R, rows)
    qr = max(d for d in (8, 4, 2, 1) if rows % d == 0)
    krb = min(qr + kr - 1, rows)
    kcb = min(NAT_QC + WIN_C - 1, GRID_W)
    n_rb, n_cb = rows // qr, GRID_W // NAT_QC
    rs = np.clip(np.arange(rows) - kr // 2, 0, rows - kr)
    cs = np.clip(np.arange(GRID_W) - WIN_C // 2, 0, GRID_W - WIN_C)
    r0 = np.clip(rs[np.arange(n_rb) * qr], 0, rows - krb)
    c0 = np.clip(cs[np.arange(n_cb) * NAT_QC], 0, GRID_W - kcb)
    q_r = (np.arange(n_rb)[:, None] * qr + np.arange(qr))[:, None, :, None, None, None]
    q_c = (np.arange(n_cb)[:, None] * NAT_QC + np.arange(NAT_QC))[None, :, None, :, None, None]
    k_r = (r0[:, None] + np.arange(krb))[:, None, None, None, :, None]
    k_c_all = c0[:, None] + np.arange(kcb)
    k_c = k_c_all[None, :, None, None, None, :]
    shape = (n_rb, n_cb, qr, NAT_QC, krb, kcb)
    mask = (k_r >= rs[q_r]) & (k_r < rs[q_r] + kr) & (k_c >= cs[q_c]) & (k_c < cs[q_c] + WIN_C)
    dr = np.clip(k_r - q_r + WIN_R - 1, 0, 2 * WIN_R - 2)
    dc = np.clip(k_c - q_c + WIN_C - 1, 0, 2 * WIN_C - 2)
    flat = lambda a: np.broadcast_to(a, shape).reshape(n_rb, n_cb, qr * NAT_QC, krb * kcb)
    return (qr, krb, kcb, r0.astype(np.int32), k_c_all.astype(np.int32),
            flat(mask), flat(dr).astype(np.int32), flat(dc).astype(np.int32))


def _natten_mixer(h_lat, h_ctx, w_in, rpb, w_out, with_ctx_out):
    bsz, n, d = h_lat.shape
    rows = n // GRID_W
    qr, krb, kcb, r0, k_cols, mask, dr, dc = _natten_layout(rows)
    n_rb, n_cb = rows // qr, GRID_W // NAT_QC
    scale = NAT_DH ** -0.5

    def heads(h):
        p = (h @ w_in).reshape(h.shape[0], h.shape[1], 3, NAT_HEADS, NAT_DH).transpose(2, 0, 3, 1, 4)
        return p[0], p[1], p[2]

    ql, kl, vl = heads(h_lat)
    qc, kc, vc = heads(h_ctx)
    k_grid = kl.reshape(bsz, NAT_HEADS, rows, GRID_W, NAT_DH)
    v_grid = vl.reshape(bsz, NAT_HEADS, rows, GRID_W, NAT_DH)
    q_blk = ql.reshape(bsz, NAT_HEADS, n_rb, qr, n_cb, NAT_QC, NAT_DH).transpose(2, 0, 1, 4, 3, 5, 6)
    q_blk = q_blk.reshape(n_rb, bsz, NAT_HEADS, n_cb, qr * NAT_QC, NAT_DH)
    col_idx = jnp.asarray(k_cols)
    kloc = krb * kcb

    def band(t_rows):
        t = t_rows[:, :, :, col_idx]
        return t.transpose(0, 1, 3, 2, 4, 5).reshape(bsz, NAT_HEADS, n_cb, kloc, NAT_DH)

    def row_block(xs):
        q_b, r_start, m_b, dr_b, dc_b = xs
        k_b = band(lax.dynamic_slice_in_dim(k_grid, r_start, krb, axis=2))
        v_b = band(lax.dynamic_slice_in_dim(v_grid, r_start, krb, axis=2))
        bias = rpb[:, dr_b, dc_b].astype(jnp.float32)
        s_loc = jnp.einsum('bhnqd,bhnkd->bhnqk', q_b, k_b).astype(jnp.float32) * scale + bias
        s_loc = jnp.where(m_b, s_loc, NEG_INF)
        s_ctx = jnp.einsum('bhnqd,bhkd->bhnqk', q_b, kc).astype(jnp.float32) * scale
        p = jax.nn.softmax(jnp.concatenate([s_loc, s_ctx], axis=-1), axis=-1).astype(v_b.dtype)
        return (jnp.einsum('bhnqk,bhnkd->bhnqd', p[..., :kloc], v_b)
                + jnp.einsum('bhnqk,bhkd->bhnqd', p[..., kloc:], vc))

    o = lax.map(row_block, (q_blk, jnp.asarray(r0), jnp.asarray(mask), jnp.asarray(dr), jnp.asarray(dc)))
    o = o.reshape(n_rb, bsz, NAT_HEADS, n_cb, qr, NAT_QC, NAT_DH).transpose(1, 0, 4, 3, 5, 2, 6)
    y_lat = o.reshape(bsz, n, d) @ w_out
    if not with_ctx_out:
        return y_lat, None
    s_c = jnp.einsum('bhqd,bhkd->bhqk', qc, kc).astype(jnp.float32) * scale
    o_c = jnp.einsum('bhqk,bhkd->bhqd', jax.nn.softmax(s_c, axis=-1).astype(vc.dtype), vc)
    y_ctx = o_c.transpose(0, 2, 1, 3).reshape(h_ctx.shape) @ w_out
    return y_lat, y_ctx


def _hier_moe(tok, w_group, b_group, w_expert, b_expert, w1, w3, w2):
    t_count, d = tok.shape
    g_logits = (tok @ w_group).astype(jnp.float32) + b_group
    g_p, g_idx = lax.top_k(jax.nn.softmax(g_logits, axis=-1), 1)
    e_logits = ((tok @ w_expert).astype(jnp.float32) + b_expert).reshape(t_count, N_GROUPS, EXPERTS_PER_GROUP)
    e_in = jnp.take_along_axis(e_logits, g_idx[:, :, None], axis=1)[:, 0]
    e_p, e_idx = lax.top_k(jax.nn.softmax(e_in, axis=-1), TOP_K_IN_GROUP)
    e_p = e_p / jnp.sum(e_p, axis=-1, keepdims=True)
    weight = (g_p * e_p).reshape(-1)
    expert = (g_idx * EXPERTS_PER_GROUP + e_idx).reshape(-1)
    token = jnp.repeat(jnp.arange(t_count, dtype=jnp.int32), TOP_K_IN_GROUP)
    n_assign = t_count * TOP_K_IN_GROUP
    order = jnp.argsort(expert)
    e_sorted = expert[order]
    counts = jnp.bincount(expert, length=N_EXPERTS)
    starts = jnp.cumsum(counts) - counts
    padded = (counts + MOE_BLOCK - 1) // MOE_BLOCK * MOE_BLOCK
    pad_end = jnp.cumsum(padded)
    pad_start = pad_end - padded
    dest = pad_start[e_sorted] + jnp.arange(n_assign) - starts[e_sorted]
    n_blocks = -(-n_assign // MOE_BLOCK) + N_EXPERTS
    cap = n_blocks * MOE_BLOCK
    slot_token = jnp.full((cap,), t_count, jnp.int32).at[dest].set(token[order])
    slot_weight = jnp.zeros((cap,), jnp.float32).at[dest].set(weight[order])
    block_expert = jnp.minimum(jnp.searchsorted(pad_end, jnp.arange(n_blocks) * MOE_BLOCK, side='right'),
                               N_EXPERTS - 1)
    tok_pad = jnp.concatenate([tok, jnp.zeros((1, d), tok.dtype)], axis=0)

    def run(xs):
        idx, e = xs
        xb = tok_pad[idx]
        return (jax.nn.silu(xb @ w1[e]) * (xb @ w3[e])) @ w2[e]

    y = lax.map(run, (slot_token.reshape(n_blocks, MOE_BLOCK), block_expert))
    y = y.reshape(cap, d).astype(jnp.float32) * slot_weight[:, None]
    out = jax.ops.segment_sum(y, slot_token, num_segments=t_count + 1)[:t_count]
    return out.astype(tok.dtype)


def setup_inputs(seed: int = 0) -> dict:
    key = jax.random.key(seed)
    ks = jax.random.split(key, 24)
    f32 = jnp.float32
    nrm = lambda k, shape, s: jax.random.normal(k, shape, f32) * s
    D = D_MODEL
    return {
        "x": nrm(ks[0], (BATCH, SEQ, D), 1.0),
        "c": nrm(ks[1], (BATCH, D), 1.0),
        "ctx": nrm(ks[2], (BATCH, CTX_LEN, D), 1.0),
        "c_ctx": nrm(ks[3], (D,), 1.0),
        "ada_w": nrm(ks[4], (DEPTH, D, 6 * D), 0.5 * D ** -0.5),
        "ada_b": nrm(ks[5], (DEPTH, 6 * D), 0.02),
        "ln_g": 1.0 + nrm(ks[6], (DEPTH, 2, D), 0.02),
        "ln_b": nrm(ks[7], (DEPTH, 2, D), 0.02),
        "gla_w_in": nrm(ks[8], (N_GLA_LAYERS, D, GLA_IN), D ** -0.5),
        "gla_w_gate": nrm(ks[9], (N_GLA_LAYERS, 2, GLA_GATE_RANK, GLA_QK), GLA_GATE_RANK ** -0.5),
        "gla_b_gate": nrm(ks[10], (N_GLA_LAYERS, 2, GLA_QK), 0.5),
        "gla_norm_g": 1.0 + nrm(ks[11], (N_GLA_LAYERS, GLA_DV), 0.02),
        "gla_w_out": nrm(ks[12], (N_GLA_LAYERS, GLA_VD, D), DEEPNORM_BETA * GLA_VD ** -0.5),
        "nat_w_in": nrm(ks[13], (N_NAT_LAYERS, D, 3 * D), D ** -0.5),
        "nat_rpb": nrm(ks[14], (N_NAT_LAYERS, NAT_HEADS, 2 * WIN_R - 1, 2 * WIN_C - 1), 0.1),
        "nat_w_out": nrm(ks[15], (N_NAT_LAYERS, D, D), DEEPNORM_BETA * D ** -0.5),
        "moe_w_group": nrm(ks[16], (DEPTH, D, N_GROUPS), D ** -0.5),
        "moe_b_group": nrm(ks[17], (DEPTH, N_GROUPS), 0.01),
        "moe_w_expert": nrm(ks[18], (DEPTH, D, N_EXPERTS), D ** -0.5),
        "moe_b_expert": nrm(ks[19], (DEPTH, N_EXPERTS), 0.01),
        "moe_w1": nrm(ks[20], (DEPTH, N_EXPERTS, D, D_EXPERT), D ** -0.5),
        "moe_w3": nrm(ks[21], (DEPTH, N_EXPERTS, D, D_EXPERT), D ** -0.5),
        "moe_w2": nrm(ks[22], (DEPTH, N_EXPERTS, D_EXPERT, D), DEEPNORM_BETA * D_EXPERT ** -0.5),
    }


def reference(x, c, ctx, c_ctx, ada_w, ada_b, ln_g, ln_b,
              gla_w_in, gla_w_gate, gla_b_gate, gla_norm_g, gla_w_out,
              nat_w_in, nat_rpb, nat_w_out,
              moe_w_group, moe_b_group, moe_w_expert, moe_b_expert, moe_w1, moe_w3, moe_w2):
    bsz, n, d = x.shape
    h_lat, h_ctx = x, ctx
    for i in range(DEPTH):
        last = i == DEPTH - 1
        j = i // N_MIXERS
        mod_l = (jax.nn.silu(c) @ ada_w[i] + ada_b[i])[:, None, :]
        mod_c = (jax.nn.silu(c_ctx) @ ada_w[i] + ada_b[i])[None, None, :]
        sh1_l, sc1_l, gt1_l, sh2_l, sc2_l, gt2_l = jnp.split(mod_l, 6, axis=-1)
        sh1_c, sc1_c, gt1_c, sh2_c, sc2_c, gt2_c = jnp.split(mod_c, 6, axis=-1)
        a_l = h_lat * (1.0 + sc1_l) + sh1_l
        a_c = h_ctx * (1.0 + sc1_c) + sh1_c
        if i % N_MIXERS == 0:
            y_l, y_c = _gla_mixer(a_l, a_c, gla_w_in[j], gla_w_gate[j], gla_b_gate[j], gla_norm_g[j], gla_w_out[j])
        else:
            y_l, y_c = _natten_mixer(a_l, a_c, nat_w_in[j], nat_rpb[j], nat_w_out[j], not last)
        h_lat = _layer_norm(DEEPNORM_ALPHA * h_lat + gt1_l * y_l, ln_g[i, 0], ln_b[i, 0])
        f_l = (h_lat * (1.0 + sc2_l) + sh2_l).reshape(-1, d)
        if last:
            y2_l = _hier_moe(f_l, moe_w_group[i], moe_b_group[i], moe_w_expert[i], moe_b_expert[i],
                             moe_w1[i], moe_w3[i], moe_w2[i]).reshape(bsz, n, d)
        else:
            h_ctx = _layer_norm(DEEPNORM_ALPHA * h_ctx + gt1_c * y_c, ln_g[i, 0], ln_b[i, 0])
            f_c = (h_ctx * (1.0 + sc2_c) + sh2_c).reshape(-1, d)
            y2 = _hier_moe(jnp.concatenate([f_l, f_c], axis=0), moe_w_group[i], moe_b_group[i],
                           moe_w_expert[i], moe_b_expert[i], moe_w1[i], moe_w3[i], moe_w2[i])
            y2_l = y2[:bsz * n].reshape(bsz, n, d)
            h_ctx = _layer_norm(DEEPNORM_ALPHA * h_ctx + gt2_c * y2[bsz * n:].reshape(h_ctx.shape),
                                ln_g[i, 1], ln_b[i, 1])
        h_lat = _layer_norm(DEEPNORM_ALPHA * h_lat + gt2_l * y2_l, ln_g[i, 1], ln_b[i, 1])
    return h_lat
```

```python
import os
import numpy as np
import ml_dtypes
from contextlib import ExitStack
import concourse.bass as bass
import concourse.mybir as mybir
from concourse.bass_utils import run_bass_kernel_spmd

F32 = mybir.dt.float32
BF16 = mybir.dt.bfloat16
AF = mybir.ActivationFunctionType
ALU = mybir.AluOpType
AX = mybir.AxisListType

D = 2048
NCH = 16
NCTX_T = 2
NLAT_T = 32
NP_T = 18
NOWN_T = 16
NT0 = NCTX_T + NLAT_T
NTP = NCTX_T + NP_T
ALPHA = 4.0 ** 0.25
EPS = 1e-5
GLA_IN = 6176
NEG = -30000.0

ENGS = ["sync", "act", "pool", "pe", "dve"]


class Buf:
    __slots__ = ("w", "r")

    def __init__(self):
        self.w = None
        self.r = {}


class Tn:
    __slots__ = ("ap", "buf")

    def __init__(self, ap, buf=None):
        self.ap = ap
        self.buf = buf if buf is not None else Buf()

    def v(self, ap):
        return Tn(ap, self.buf)


class Sched:
    def __init__(self):
        self.streams = {e: [] for e in ENGS}
        self.count = {}
        self.waited = {e: {} for e in ENGS}

    def op(self, eng, fn, reads=(), writes=(), inc=True, dma=None):
        deps = {}

        def need(tok):
            if tok is not None:
                k, v = tok
                if deps.get(k, 0) < v:
                    deps[k] = v

        for t in reads:
            need(t.buf.w)
        for t in writes:
            need(t.buf.w)
            for k, v in t.buf.r.items():
                need((k, v))
        st = self.streams[eng]
        wd = self.waited[eng]
        for k, v in deps.items():
            if eng == "pe" and k == "c_pe":
                continue
            if wd.get(k, 0) >= v:
                continue
            st.append(("w", k, v))
            wd[k] = v
        if dma is not None:
            key = "d_" + dma
            self.count[key] = self.count.get(key, 0) + 16
            tok = (key, self.count[key])
            st.append(("o", fn, key, 16))
        else:
            key = "c_" + eng
            if inc:
                self.count[key] = self.count.get(key, 0) + 1
                tok = (key, self.count[key])
                st.append(("o", fn, key, 1))
            else:
                tok = (key, self.count.get(key, 0) + 1)
                st.append(("o", fn, None, 0))
        for t in reads:
            k, v = tok
            if t.buf.r.get(k, 0) < v:
                t.buf.r[k] = v
        for t in writes:
            t.buf.w = tok
            t.buf.r = {}
        return tok

    def barrier(self):
        for e in ENGS:
            wd = self.waited[e]
            for k, v in self.count.items():
                if e == "pe" and k == "c_pe":
                    continue
                if wd.get(k, 0) < v:
                    self.streams[e].append(("w", k, v))
                    wd[k] = v

    def emit(self, nc):
        with ExitStack() as es:
            sems = {k: es.enter_context(nc.semaphore(k)) for k in self.count}
            block = es.enter_context(nc.Block())

            def runner(name):
                def f(eng):
                    for it in self.streams[name]:
                        if it[0] == "w":
                            eng.wait_ge(sems[it[1]], it[2])
                        else:
                            ins = it[1](eng)
                            if it[2] is not None:
                                ins.then_inc(sems[it[2]], it[3])
                return f

            block.sync(runner("sync"))
            block.scalar(runner("act"))
            block.gpsimd(runner("pool"))
            block.tensor(runner("pe"))
            block.vector(runner("dve"))


class Arena:
    def __init__(self, t, n):
        self.t = t
        self.n = n
        self.off = 0

    def reset(self):
        self.off = 0

    def f32(self, cols):
        a = self.off
        self.off += cols
        assert self.off <= self.n, ("arena overflow", self.off, self.n)
        return Tn(self.t[:, a:a + cols])

    def bf16(self, cols):
        c = (cols + 1) // 2
        a = self.off
        self.off += c
        assert self.off <= self.n, ("arena overflow", self.off, self.n)
        return Tn(self.t[:, a:a + c].bitcast(BF16))


C_ID = 0
C_LA = 128
C_RA = 256
C_LB = 384
C_RB = 512
C_MA = 640
C_MB = 768
C_NEG = 896
C_ONE = 897
C_SLT = 1025
C_P4 = 1153
C_PC = 1157
NCONST = 1165
MOE_B = 512
I32 = mybir.dt.int32


def make_consts():
    c = np.zeros((128, NCONST), np.float32)
    i = np.arange(128)
    tp, t = i[:, None], i[None, :]
    c[:, C_ID:C_ID + 128] = (tp == t)
    c[:, C_LA:C_LA + 128] = (tp <= t) * (-1.0 / 16)
    c[:, C_RA:C_RA + 128] = (tp > t) * (-1.0 / 16)
    c[:, C_LB:C_LB + 128] = (tp >= t) * (-1.0 / 16)
    c[:, C_RB:C_RB + 128] = (tp < t) * (-1.0 / 16)
    c[:, C_MA:C_MA + 128] = (tp <= t)
    c[:, C_MB:C_MB + 128] = (tp >= t)
    c[:, C_NEG] = -1.0 / 16
    c[:, C_ONE:C_ONE + 128] = 1.0
    c[:, C_SLT:C_SLT + 128] = (tp < t)
    for q in range(4):
        c[:, C_P4 + q] = 4 * i + q
    for cc in range(8):
        c[:, C_PC + cc] = 128 * cc + i
    return c


class Prog:
    def __init__(self, debug=None):
        self.debug = debug
        self.nc = bass.Bass("TRN2", target_bir_lowering=False)
        self.S = Sched()
        self.es = ExitStack()
        self.dram = {}
        self.dbufs = {}
        self.outs = []
        self._bc = 0

    def din(self, name, shape, dt=F32):
        h = self.nc.dram_tensor(name, list(shape), dt, kind="ExternalInput")
        self.dram[name] = h.ap()
        return self.dram[name]

    def dout(self, name, shape, dt=F32):
        h = self.nc.dram_tensor(name, list(shape), dt, kind="ExternalOutput")
        self.dram[name] = h.ap()
        return self.dram[name]

    def dscr(self, name, shape, dt=F32):
        if self.debug and name in self.debug:
            return self.dout(name, shape, dt)
        h = self.nc.dram_tensor(name, list(shape), dt)
        self.dram[name] = h.ap()
        return self.dram[name]

    def db(self, name, idx=0):
        k = (name, idx)
        if k not in self.dbufs:
            self.dbufs[k] = Tn(None)
        return self.dbufs[k]

    def dma(self, eng, out_ap, in_ap, reads, writes, tag):
        self.S.op(eng, lambda e: e.dma_start(out=out_ap, in_=in_ap), reads=reads, writes=writes, dma=tag)

    def mm(self, out, lhsT, rhs, start, stop, reads, writes):
        self.S.op("pe", lambda e: e.matmul(out, lhsT, rhs, start=start, stop=stop), reads=reads, writes=writes, inc=stop)

    def tr(self, out, in_, ident, reads, writes, inc=True):
        self.S.op("pe", lambda e: e.transpose(out, in_, ident), reads=reads, writes=writes, inc=inc)

    def tt(self, eng, out, in0, in1, op, reads, writes):
        self.S.op(eng, lambda e: e.tensor_tensor(out=out, in0=in0, in1=in1, op=op), reads=reads, writes=writes)

    def cp(self, eng, out, in_, reads, writes):
        if eng == "act":
            self.S.op(eng, lambda e: e.copy(out, in_), reads=reads, writes=writes)
        else:
            self.S.op(eng, lambda e: e.tensor_copy(out, in_), reads=reads, writes=writes)

    def act(self, out, in_, func, reads, writes, scale=None, bias=None, accum=None):
        kw = {}
        if scale is not None:
            kw["scale"] = scale
        if bias is not None:
            kw["bias"] = bias
        if accum is not None:
            kw["accum_out"] = accum
        self.S.op("act", lambda e: e.activation(out=out, in_=in_, func=func, **kw), reads=reads, writes=writes)

    def stt(self, eng, out, in0, scalar, in1, op0, op1, reads, writes):
        self.S.op(eng, lambda e: e.scalar_tensor_tensor(out=out, in0=in0, scalar=scalar, in1=in1, op0=op0, op1=op1), reads=reads, writes=writes)

    def ts(self, eng, out, in0, s1, s2, op0, op1, reads, writes):
        if s2 is None:
            self.S.op(eng, lambda e: e.tensor_scalar(out=out, in0=in0, scalar1=s1, scalar2=None, op0=op0), reads=reads, writes=writes)
        else:
            self.S.op(eng, lambda e: e.tensor_scalar(out=out, in0=in0, scalar1=s1, scalar2=s2, op0=op0, op1=op1), reads=reads, writes=writes)

    def ms(self, eng, ap, val, writes):
        self.S.op(eng, lambda e: e.memset(ap, val), writes=writes)

    def sb(self, shape, dt, name):
        return self.es.enter_context(self.nc.sbuf_tensor(name, list(shape), dt))

    def setup(self):
        nc = self.nc
        self.arena_t = self.sb([128, 50000], F32, "arena")
        self.A = Arena(self.arena_t, 50000)
        self.consts = Tn(self.sb([128, NCONST], F32, "consts"))
        self.identb = Tn(self.sb([128, 128], BF16, "identb"))
        self.modT = Tn(self.sb([128, 2 * 2 * 96], F32, "modT"))
        self.onepT = Tn(self.sb([128, 2 * 2 * 2 * 16], F32, "onepT"))
        self.wt_all = Tn(self.sb([128, NTP * 16], F32, "wt_all"))
        self.A_all = Tn(self.sb([128, NTP * 16], F32, "A_all"))
        self.didx = Tn(self.sb([128, NTP * 2], I32, "didx"))
        self.didx2 = Tn(self.sb([128, NTP * 2], I32, "didx2"))
        self.dw = Tn(self.sb([128, NTP * 2], F32, "dw"))
        self.widx = Tn(self.sb([128, 26 * 12], I32, "widx"))
        self.small = Tn(self.sb([128, 64], F32, "small"))
        self.ps = [Tn(self.es.enter_context(nc.psum_tensor("ps%d" % i, [128, 512], F32))) for i in range(8)]
        cin = self.din("consts_in", [128, NCONST])
        self.dma("sync", self.consts.ap[:, :], cin[:, :], [], [self.consts], "consts")
        self.S.op("dve", lambda e: e.tensor_copy(self.identb.ap[:, :], self.consts.ap[:, C_ID:C_ID + 128]),
                  reads=[self.consts], writes=[self.identb])

    def ident(self, n=128):
        return self.consts.ap[0:n, C_ID:C_ID + n]

    def phase_mod(self):
        S, A = self.S, self.A
        cc = self.din("cc", [2, D])
        ada_w = self.din("ada_w", [2, D, 6 * D])
        ada_b = self.din("ada_b", [2, 6 * D])
        modrow = self.dscr("modrow", [2, 2, 6 * D])
        A.reset()
        cct = A.f32(D)
        sct = A.f32(D)
        scT = A.bf16(32)
        adab = A.f32(6 * D)
        mrow = A.f32(6 * D)
        wsl = [A.bf16(16 * 512) for _ in range(2)]
        self.dma("sync", cct.ap[0:2, :], cc[:, :], [], [cct], "cc")
        S.op("act", lambda e: e.activation(out=sct.ap[0:2, :], in_=cct.ap[0:2, :], func=AF.Silu), reads=[cct], writes=[sct])
        p0 = self.ps[0]
        for k in range(NCH):
            self.tr(p0.ap[:, 2 * k:2 * k + 2], sct.ap[0:2, k * 128:(k + 1) * 128], self.ident(2), [sct, self.consts], [p0], inc=(k == NCH - 1))
        S.op("dve", lambda e: e.tensor_copy(scT.ap[:, 0:32], p0.ap[:, 0:32]), reads=[p0], writes=[scT])
        n = 0
        for l in range(2):
            for r in range(2):
                self.dma("sync", adab.ap[r:r + 1, :], ada_b[l:l + 1, :], [], [adab], "adab")
            wv = ada_w[l].rearrange("(k p) n -> p k n", p=128)
            for s in range(24):
                w = wsl[n % 2]
                pb = self.ps[1 + n % 2]
                n += 1
                wap = w.ap.rearrange("p (k n) -> p k n", k=16)
                self.dma("pool", wap, wv[:, :, s * 512:(s + 1) * 512], [], [w], "wsl%d" % (n % 2))
                for k in range(NCH):
                    self.mm(pb.ap[0:2, :], scT.ap[:, 2 * k:2 * k + 2], wap[:, k, :], k == 0, k == NCH - 1, [scT, w], [pb])
                sl = slice(s * 512, (s + 1) * 512)
                S.op("dve", lambda e, pb=pb, sl=sl: e.tensor_tensor(out=mrow.ap[0:2, sl], in0=pb.ap[0:2, :], in1=adab.ap[0:2, sl], op=ALU.add),
                     reads=[pb, adab], writes=[mrow])
            self.dma("sync", modrow[l], mrow.ap[0:2, :], [mrow], [self.db("modrow", l)], "mrow")
        mr = A.f32(128)
        for l in range(2):
            for kind in range(2):
                self.dma("sync", mr.ap[0:96, :], modrow[l, kind].rearrange("(c p) -> c p", p=128), [self.db("modrow", l)], [mr], "mr")
                pb = self.ps[3]
                self.tr(pb.ap[:, 0:96], mr.ap[0:96, :], self.ident(96), [mr, self.consts], [pb])
                o = (l * 2 + kind) * 96
                S.op("dve", lambda e, o=o, pb=pb: e.tensor_copy(self.modT.ap[:, o:o + 96], pb.ap[:, 0:96]), reads=[pb], writes=[self.modT])
                for wi, v in enumerate((1, 4)):
                    oo = ((l * 2 + kind) * 2 + wi) * 16
                    S.op("dve", lambda e, o=o, oo=oo, v=v: e.tensor_scalar_add(self.onepT.ap[:, oo:oo + 16], self.modT.ap[:, o + v * 16:o + v * 16 + 16], 1.0),
                         reads=[self.modT], writes=[self.onepT])
        S.barrier()

    def mcol(self, l, kind, v, k):
        o = (l * 2 + kind) * 96 + v * 16 + k
        return self.modT.ap[:, o:o + 1]

    def ocol(self, l, kind, wi, k):
        o = ((l * 2 + kind) * 2 + wi) * 16 + k
        return self.onepT.ap[:, o:o + 1]

    def build_aT(self, aT, tiles, src_fn, l, wi, vsh, xs, gm=False):
        S = self.S
        for i, g in enumerate(tiles):
            kind = 1 if g < NCTX_T else 0
            x = xs[i % 2]
            src, dep = src_fn(g)
            self.dma("sync", x.ap[:, :], src, [dep] if dep is not None else [], [x], "xs%d" % (i % 2))
            for q in range(4):
                pb = self.ps[4 + (i * 4 + q) % 4]
                for j in range(4):
                    k = q * 4 + j
                    self.tr(pb.ap[:, j * 128:(j + 1) * 128], x.ap[:, k * 128:(k + 1) * 128], self.ident(), [x, self.consts], [pb], inc=(j == 3))
                for j in range(4):
                    k = q * 4 + j
                    o = (((i // 4) * 16 + k) * 512 + (i % 4) * 128) if gm else (i * 16 + k) * 128
                    S.op("act", lambda e, pb=pb, j=j, o=o, kind=kind, k=k: e.activation(
                        out=aT.ap[:, o:o + 128], in_=pb.ap[:, j * 128:(j + 1) * 128], func=AF.Identity,
                        scale=self.ocol(l, kind, wi, k), bias=self.mcol(l, kind, vsh, k)),
                        reads=[pb, self.onepT, self.modT], writes=[aT])

    def proj_tm(self, aT, ntiles, w_dram, col0, ncols, wsl, sink, skip=None):
        wv = w_dram.rearrange("(k p) n -> p k n", p=128)
        nb = (ncols + 511) // 512
        n = 0
        for cb in range(nb):
            c0 = col0 + cb * 512
            cw = min(512, col0 + ncols - c0)
            w = wsl[cb % 2]
            wap = w.ap.rearrange("p (k n) -> p k n", k=16)
            self.dma("pool", wap[:, :, 0:cw], wv[:, :, c0:c0 + cw], [], [w], "wsl%d" % (cb % 2))
            for i in range(ntiles):
                if skip is not None and skip(i, cb):
                    continue
                pb = self.ps[n % 4]
                n += 1
                for k in range(NCH):
                    o = (i * 16 + k) * 128
                    self.mm(pb.ap[:, 0:cw], aT.ap[:, o:o + 128], wap[:, k, 0:cw], k == 0, k == NCH - 1, [aT, w], [pb])
                sink(i, cb, c0, cw, pb)

    def phase_gla_inproj(self):
        S, A = self.S, self.A
        x = self.din("x", [NLAT_T * 128, D])
        ctx = self.din("ctx", [NCTX_T * 128, D])
        w_in = self.din("gla_w_in", [D, GLA_IN])
        big = self.dscr("big", [2 * 26 * MOE_B * 2048], BF16)
        proj = big[0:NT0 * 128 * GLA_IN * 2].bitcast(F32).rearrange("(a p n) -> a p n", a=NT0, p=128)
        self.dram["proj"] = proj
        big2d = big.rearrange("(r c) -> r c", c=2048)
        self.dram["big2d"] = big2d
        self.dram["Xs"] = big2d[0:26 * MOE_B, :]
        self.dram["Yd"] = big2d[26 * MOE_B:2 * 26 * MOE_B, :]

        def src(g):
            if g < NCTX_T:
                return ctx[g * 128:(g + 1) * 128, :], None
            return x[(g - NCTX_T) * 128:(g - NCTX_T + 1) * 128, :], None

        for grp in range(2):
            tiles = list(range(grp * 17, grp * 17 + 17))
            A.reset()
            aT = A.bf16(17 * 16 * 128)
            xs = [A.f32(D) for _ in range(2)]
            self.build_aT(aT, tiles, src, 0, 0, 0, xs)
            wsl = [A.bf16(16 * 512) for _ in range(2)]
            stg = [A.f32(512) for _ in range(4)]
            cnt = [0]

            def sink(i, cb, c0, cw, pb):
                n = cnt[0]
                cnt[0] += 1
                st = stg[n % 4]
                if n % 2 == 0:
                    S.op("dve", lambda e: e.tensor_copy(st.ap[:, 0:cw], pb.ap[:, 0:cw]), reads=[pb], writes=[st])
                else:
                    S.op("act", lambda e: e.copy(st.ap[:, 0:cw], pb.ap[:, 0:cw]), reads=[pb], writes=[st])
                g = tiles[i]
                self.dma("sync", proj[g, :, c0:c0 + cw], st.ap[:, 0:cw], [st], [self.db("proj", g)], "stg%d" % (n % 4))

            self.proj_tm(aT, 17, w_in, 0, GLA_IN, wsl, sink, skip=lambda i, cb: tiles[i] >= NTP and (cb < 2 or 8 <= cb < 12))
            S.barrier()

    def bcast_row(self, dst, dst_ap, src_ap, n, dep, rowbuf, tag):
        S = self.S
        self.dma("sync", rowbuf.ap[0:1, 0:n], src_ap, [dep] if dep is not None else [], [rowbuf], tag)
        for c0 in range(0, n, 512):
            cw = min(512, n - c0)
            pb = self.ps[self._bc % 2]
            self._bc += 1
            self.mm(pb.ap[:, 0:cw], self.consts.ap[0:1, C_ONE:C_ONE + 128], rowbuf.ap[0:1, c0:c0 + cw], True, True, [self.consts, rowbuf], [pb])
            S.op("dve", lambda e, pb=pb, c0=c0, cw=cw: e.tensor_copy(dst_ap[:, c0:c0 + cw], pb.ap[:, 0:cw]), reads=[pb], writes=[dst])

    def phase_gla_prep(self):
        import math
        S, A = self.S, self.A
        proj = self.dram["proj"]
        wg_in = self.din("gla_wg", [2, 17, 1024])
        cos_in = self.din("rope_cos", [NT0, 128, 512])
        sin_in = self.din("rope_sin", [NT0, 128, 512])
        qdT = self.dscr("qdT", [2, NT0, 128, 1024], BF16)
        kiT = self.dscr("kiT", [2, NT0, 128, 1024], BF16)
        ke = self.dscr("ke", [2, NT0, 128, 1024], BF16)
        vsc = self.dscr("vsc", [NT0, 128, 2048], BF16)
        sr = self.dscr("sr", [NTP, 128, 2048], BF16)
        A.reset()
        self.decay = A.f32(2 * NT0 * 8)
        arena_keep = A.off
        wg = A.f32(2 * 1024)
        pj = [A.f32(GLA_IN) for _ in range(2)]
        cs = [A.f32(512) for _ in range(2)]
        sn = [A.f32(512) for _ in range(2)]
        qrs = [A.f32(1024) for _ in range(2)]
        krs = [A.f32(1024) for _ in range(2)]
        t1 = A.f32(512)
        t2 = A.f32(512)
        t3 = A.f32(512)
        t4 = A.f32(512)
        glT = [A.f32(128) for _ in range(2)]
        exs = [A.f32(1024) for _ in range(2)]
        sps = [A.f32(1024) for _ in range(2)]
        Es = [[A.f32(1024) for _ in range(3)] for _ in range(2)]
        ob = [[A.bf16(1024) for _ in range(3)] for _ in range(2)]
        tT = [[A.bf16(1024) for _ in range(2)] for _ in range(2)]
        vb = [A.bf16(2048) for _ in range(2)]
        rb = [A.bf16(2048) for _ in range(2)]
        self.dma("sync", wg.ap[0:17, :].rearrange("p (d n) -> p d n", d=2), wg_in.rearrange("d p n -> p d n"), [], [wg], "wg")
        for d in range(2):
            self.ms("pool", glT[d].ap[0:17, :], 1.0, [glT[d]])
        psz = [self.ps[0], self.ps[1]]
        pcum = [self.ps[2], self.ps[3]]
        prem = [self.ps[4], self.ps[5]]
        ptr = self.ps[6]
        ptot = self.ps[7]
        ptr_b = ptr.ap.bitcast(BF16)
        lnsc = math.log(1.0 / 16.0)
        for g in range(NT0):
            p = pj[g % 2]
            qr, kr = qrs[g % 2], krs[g % 2]
            c_, s_ = cs[g % 2], sn[g % 2]
            self.dma("sync", p.ap[:, :], proj[g], [self.db("proj", g)], [p], "pj%d" % (g % 2))
            self.dma("sync", c_.ap[:, :], cos_in[g], [], [c_], "cs%d" % (g % 2))
            self.dma("sync", s_.ap[:, :], sin_in[g], [], [s_], "sn%d" % (g % 2))
            cv = c_.ap.rearrange("p (a f) -> p a f", a=8)
            sv = s_.ap.rearrange("p (a f) -> p a f", a=8)
            full = g < NTP
            for (eng, src0, dstt, ta, tb) in (("dve", 0, qr, t1, t2), ("pool", 1024, kr, t3, t4)):
                if src0 == 0 and not full:
                    continue
                xv = p.ap[:, src0:src0 + 1024].rearrange("p (a h f) -> p a h f", a=8, h=2)
                dv = dstt.ap.rearrange("p (a h f) -> p a h f", a=8, h=2)
                x1, x2 = xv[:, :, 0, :], xv[:, :, 1, :]
                tav = ta.ap.rearrange("p (a f) -> p a f", a=8)
                tbv = tb.ap.rearrange("p (a f) -> p a f", a=8)
                self.tt(eng, tav, x1, cv, ALU.mult, [p, c_], [ta])
                self.tt(eng, tbv, x2, sv, ALU.mult, [p, s_], [tb])
                self.tt(eng, dv[:, :, 0, :], tav, tbv, ALU.subtract, [ta, tb], [dstt])
                self.tt(eng, tav, x1, sv, ALU.mult, [p, s_], [ta])
                self.tt(eng, tbv, x2, cv, ALU.mult, [p, c_], [tb])
                self.tt(eng, dv[:, :, 1, :], tav, tbv, ALU.add, [ta, tb], [dstt])
            vv = vb[g % 2]
            self.cp("pool", vv.ap[:, :], p.ap[:, 2048:4096], [p], [vv])
            self.dma("sync", vsc[g], vv.ap[:, :], [vv], [self.db("vsc", g)], "vb%d" % (g % 2))
            if g < NTP:
                rr = rb[g % 2]
                self.act(rr.ap[:, :], p.ap[:, 4096:6144], AF.Silu, [p], [rr])
                self.dma("sync", sr[g], rr.ap[:, :], [rr], [self.db("sr", g)], "rb%d" % (g % 2))
            for d in range(2):
                if d == 0 and not full:
                    continue
                gl = glT[d]
                ex, sp, E = exs[d], sps[d], Es[d]
                self.tr(ptot.ap[0:16, 0:128], p.ap[:, 6144 + 16 * d:6160 + 16 * d], self.ident(), [p, self.consts], [ptot])
                self.cp("dve", gl.ap[0:16, :], ptot.ap[0:16, 0:128], [ptot], [gl])
                for hh in range(2):
                    self.mm(psz[hh].ap[:, :], gl.ap[0:17, :], wg.ap[0:17, d * 1024 + hh * 512:d * 1024 + hh * 512 + 512], True, True, [gl, wg], [psz[hh]])
                for hh in range(2):
                    self.act(ex.ap[:, hh * 512:(hh + 1) * 512], psz[hh].ap[:, :], AF.Exp, [psz[hh]], [ex], scale=-1.0)
                self.act(sp.ap[:, :], ex.ap[:, :], AF.Ln, [ex], [sp], bias=1.0)
                cL = (C_LA, C_LB)[d]
                cR = (C_RA, C_RB)[d]
                for hh in range(2):
                    if full:
                        self.mm(pcum[hh].ap[:, :], self.consts.ap[:, cL:cL + 128], sp.ap[:, hh * 512:(hh + 1) * 512], True, True, [self.consts, sp], [pcum[hh]])
                    self.mm(prem[hh].ap[:, :], self.consts.ap[:, cR:cR + 128], sp.ap[:, hh * 512:(hh + 1) * 512], True, True, [self.consts, sp], [prem[hh]])
                for j in range(8):
                    self.mm(ptot.ap[:, 256 + j:257 + j], sp.ap[:, j * 128:(j + 1) * 128], self.consts.ap[:, C_NEG:C_NEG + 1], True, True, [sp, self.consts], [ptot])
                do = (d * NT0 + g) * 8
                self.act(self.decay.ap[:, do:do + 8], ptot.ap[:, 256:264], AF.Exp, [ptot], [self.decay])
                for hh in range(2):
                    sl = slice(hh * 512, (hh + 1) * 512)
                    if full:
                        self.act(E[0].ap[:, sl], pcum[hh].ap[:, :], AF.Exp, [pcum[hh]], [E[0]], bias=lnsc)
                        self.act(E[1].ap[:, sl], pcum[hh].ap[:, :], AF.Exp, [pcum[hh]], [E[1]], scale=-1.0)
                    self.act(E[2].ap[:, sl], prem[hh].ap[:, :], AF.Exp, [prem[hh]], [E[2]])
                qd_, ki_, ke_ = ob[d]
                if full:
                    self.tt("dve", qd_.ap[:, :], qr.ap[:, :], E[0].ap[:, :], ALU.mult, [qr, E[0]], [qd_])
                    self.tt("dve", ki_.ap[:, :], kr.ap[:, :], E[1].ap[:, :], ALU.mult, [kr, E[1]], [ki_])
                self.tt("pool", ke_.ap[:, :], kr.ap[:, :], E[2].ap[:, :], ALU.mult, [kr, E[2]], [ke_])
                self.dma("sync", ke[d, g], ke_.ap[:, :], [ke_], [self.db("ke%d" % d, g)], "ke%d" % d)
                for (srcb, dstT, dr, nm) in (((qd_, tT[d][0], qdT, "qdT"), (ki_, tT[d][1], kiT, "kiT")) if full else ()):
                    for j in range(8):
                        self.tr(ptr_b[:, j * 128:(j + 1) * 128], srcb.ap[:, j * 128:(j + 1) * 128], self.identb.ap[:, :], [srcb, self.identb], [ptr], inc=(j == 7))
                    self.cp("dve", dstT.ap[:, :], ptr_b[:, :], [ptr], [dstT])
                    self.dma("sync", dr[d, g], dstT.ap[:, :], [dstT], [self.db("%s%d" % (nm, d), g)], "%s%d" % (nm, d))
        S.barrier()
        return arena_keep

    def phase_gla_scan(self, keep):
        S, A = self.S, self.A
        qdT, kiT, ke, vsc, sr = (self.dram[n] for n in ("qdT", "kiT", "ke", "vsc", "sr"))
        ng_in = self.din("gla_norm_g", [1, 512])
        oA = self.dscr("oA", [NTP, 128, 2048])
        og = self.dscr("og", [NTP, 128, 2048], BF16)
        A.off = keep
        rowbuf = A.f32(2048)
        ngb = A.f32(512)
        kEb = [A.bf16(1024) for _ in range(2)]
        Vb = [A.bf16(2048) for _ in range(2)]
        QTb = [A.bf16(1024) for _ in range(2)]
        KTb = [A.bf16(1024) for _ in range(2)]
        OAb = [A.f32(2048) for _ in range(2)]
        SRb = [A.bf16(2048) for _ in range(2)]
        st = [A.f32(1024) for _ in range(4)]
        stb = [A.bf16(1024) for _ in range(4)]
        oasb = [A.f32(2048) for _ in range(2)]
        ogsb = [A.bf16(2048) for _ in range(2)]
        osum = [A.f32(512) for _ in range(2)]
        tmp = [A.f32(512) for _ in range(2)]
        junks = [A.f32(512) for _ in range(2)]
        smalls = [A.f32(4) for _ in range(4)]
        attm = [A.bf16(128) for _ in range(2)]
        self.bcast_row(ngb, ngb.ap, ng_in[0:1, :], 512, None, rowbuf, "rowbuf")
        n = 0
        for d in range(2):
            order = list(range(NTP)) if d == 0 else [1, 0] + list(range(NT0 - 1, NCTX_T - 1, -1))
            cM = (C_MA, C_MB)[d]
            for h in range(4):
                self.ms("pool", st[h].ap[:, :], 0.0, [st[h]])
                self.ms("pool", stb[h].ap[:, :], 0.0, [stb[h]])
            for g in order:
                out = g < NTP
                b = n % 2
                n += 1
                kE, V, QT, KT, OA, SR = kEb[b], Vb[b], QTb[b], KTb[b], OAb[b], SRb[b]
                self.dma("sync", kE.ap[:, :], ke[d, g], [self.db("ke%d" % d, g)], [kE], "kE%d" % b)
                self.dma("sync", V.ap[:, :], vsc[g], [self.db("vsc", g)], [V], "V%d" % b)
                if out:
                    self.dma("sync", QT.ap[:, :], qdT[d, g], [self.db("qdT%d" % d, g)], [QT], "QT%d" % b)
                    self.dma("sync", KT.ap[:, :], kiT[d, g], [self.db("kiT%d" % d, g)], [KT], "KT%d" % b)
                    if d == 1:
                        self.dma("sync", OA.ap[:, :], oA[g], [self.db("oA", g)], [OA], "OA%d" % b)
                        self.dma("sync", SR.ap[:, :], sr[g], [self.db("sr", g)], [SR], "SR%d" % b)
                oas, ogs = oasb[b], ogsb[b]
                for h in range(4):
                    sel = h % 2
                    patt, po, pu = self.ps[4 * sel], self.ps[4 * sel + 1], [self.ps[4 * sel + 2], self.ps[4 * sel + 3]]
                    hc = slice(h * 512, (h + 1) * 512)
                    if out:
                        for dc in range(2):
                            cc = slice((2 * h + dc) * 128, (2 * h + dc + 1) * 128)
                            self.mm(patt.ap[:, 0:128], KT.ap[:, cc], QT.ap[:, cc], dc == 0, dc == 1, [KT, QT], [patt])
                        am = attm[h % 2]
                        self.tt("dve", am.ap[:, :], patt.ap[:, 0:128], self.consts.ap[:, cM:cM + 128], ALU.mult, [patt, self.consts], [am])
                        self.mm(po.ap[:, :], am.ap[:, :], V.ap[:, hc], True, False, [am, V], [po])
                        for dc in range(2):
                            cc = slice((2 * h + dc) * 128, (2 * h + dc + 1) * 128)
                            self.mm(po.ap[:, :], QT.ap[:, cc], stb[h].ap[:, dc * 512:(dc + 1) * 512], False, dc == 1, [QT, stb[h]], [po])
                        if d == 0:
                            self.cp("act", oas.ap[:, hc], po.ap[:, :], [po], [oas])
                        else:
                            os_, tp_ = osum[h % 2], tmp[h % 2]
                            smh = smalls[h]
                            junk = junks[h % 2]
                            sq = smh.ap[:, 0:1]
                            rs = smh.ap[:, 1:2]
                            self.tt("dve", os_.ap[:, :], po.ap[:, :], OA.ap[:, hc], ALU.add, [po, OA], [os_])
                            self.act(junk.ap[:, :], os_.ap[:, :], AF.Square, [os_], [junk, smh], accum=sq)
                            self.act(rs, sq, AF.Ln, [smh], [smh], scale=1.0 / 512, bias=EPS)
                            self.act(rs, rs, AF.Exp, [smh], [smh], scale=-0.5)
                            self.stt("dve", tp_.ap[:, :], os_.ap[:, :], rs, ngb.ap[:, :], ALU.mult, ALU.mult, [os_, smh, ngb], [tp_])
                            self.tt("pool", ogs.ap[:, hc], tp_.ap[:, :], SR.ap[:, hc], ALU.mult, [tp_, SR], [ogs])
                    for dc in range(2):
                        cc = slice((2 * h + dc) * 128, (2 * h + dc + 1) * 128)
                        self.mm(pu[dc].ap[:, :], kE.ap[:, cc], V.ap[:, hc], True, True, [kE, V], [pu[dc]])
                        do = (d * NT0 + g) * 8 + 2 * h + dc
                        ss = slice(dc * 512, (dc + 1) * 512)
                        self.stt("dve", st[h].ap[:, ss], st[h].ap[:, ss], self.decay.ap[:, do:do + 1], pu[dc].ap[:, :], ALU.mult, ALU.add,
                                 [st[h], self.decay, pu[dc]], [st[h]])
                        self.cp("act", stb[h].ap[:, ss], st[h].ap[:, ss], [st[h]], [stb[h]])
                if out:
                    if d == 0:
                        self.dma("sync", oA[g], oas.ap[:, :], [oas], [self.db("oA", g)], "oas%d" % b)
                    else:
                        self.dma("sync", og[g], ogs.ap[:, :], [ogs], [self.db("og", g)], "ogs%d" % b)
        S.barrier()

    def phase_outproj(self, l, w_out, tiles, xsrc, lng_in, lnb_in, wr_in, br_in):
        S, A = self.S, self.A
        og = self.dram["og"]
        modrow = self.dram["modrow"]
        if "h1" not in self.dram:
            self.dscr("h1", [NTP, 128, 2048])
            self.dscr("ftm", [NTP, 128, 2048], BF16)
        h1d, ftm = self.dram["h1"], self.dram["ftm"]
        A.reset()
        wo = A.bf16(16 * 2048)
        rowbuf = A.f32(2048)
        gtb = [A.f32(2048) for _ in range(2)]
        lngb = A.f32(2048)
        lnbb = A.f32(2048)
        ogb = [A.bf16(2048)] * 2
        ogT = [A.bf16(2048) for _ in range(2)]
        xt = [A.f32(2048)] * 2
        tt_ = A.f32(2048)
        h1 = [A.f32(2048) for _ in range(2)]
        fTb = [A.bf16(2048)] * 2
        fT32 = A.f32(2048)
        opb = [A.f32(2048) for _ in range(2)]
        shb = [A.f32(2048) for _ in range(2)]
        wr = A.f32(16 * 20)
        br = A.f32(20)
        lg = A.f32(64)
        stats = A.f32(4 * 6)
        wov = wo.ap.rearrange("p (k n) -> p k n", k=16)
        for cb in range(4):
            self.dma("pool", wov[:, :, cb * 512:(cb + 1) * 512], w_out.rearrange("(k p) n -> p k n", p=128)[:, :, cb * 512:(cb + 1) * 512], [], [wo], "wo")
        self.dma("sync", wr.ap.rearrange("p (k n) -> p k n", k=16), wr_in.rearrange("(k p) n -> p k n", p=128), [], [wr], "wr")
        self.dma("sync", br.ap[0:1, :], br_in, [], [br], "br")
        kinds = sorted(set(1 if g < NCTX_T else 0 for g in tiles))
        for kind in kinds:
            self.bcast_row(gtb[kind], gtb[kind].ap, modrow[l, kind:kind + 1, 2 * D:3 * D], D, self.db("modrow", l), rowbuf, "rowbuf")
        self.bcast_row(lngb, lngb.ap, lng_in, D, None, rowbuf, "rowbuf")
        self.bcast_row(lnbb, lnbb.ap, lnb_in, D, None, rowbuf, "rowbuf")
        for kind in kinds:
            self.bcast_row(opb[kind], opb[kind].ap, modrow[l, kind:kind + 1, 4 * D:5 * D], D, self.db("modrow", l), rowbuf, "rowbuf")
            self.ts("dve", opb[kind].ap[:, :], opb[kind].ap[:, :], 1.0, None, ALU.add, None, [opb[kind]], [opb[kind]])
            self.bcast_row(shb[kind], shb[kind].ap, modrow[l, kind:kind + 1, 3 * D:4 * D], D, self.db("modrow", l), rowbuf, "rowbuf")
        sm = self.small.ap
        cut = int(os.environ.get("OP_CUT", 99))
        for i, g in enumerate(tiles):
            if cut < 1:
                break
            kind = 1 if g < NCTX_T else 0
            b = i % 2
            o_, oT, x_, h_, f_ = ogb[b], ogT[b], xt[b], h1[b], fTb[b]
            self.dma("sync", o_.ap[:, :], og[g], [self.db("og", g)], [o_], "ogb%d" % b)
            src, dep = xsrc(g)
            self.dma("sync", x_.ap[:, :], src, [dep] if dep is not None else [], [x_], "xt%d" % b)
            for half in range(2):
                pb = self.ps[4 + half]
                pbb = pb.ap.bitcast(BF16)
                for j in range(8):
                    k = half * 8 + j
                    self.tr(pbb[:, j * 128:(j + 1) * 128], o_.ap[:, k * 128:(k + 1) * 128], self.identb.ap[:, :], [o_, self.identb], [pb], inc=(j == 7))
                self.cp("act" if half else "dve", oT.ap[:, half * 1024:(half + 1) * 1024], pbb[:, :], [pb], [oT])
            for cb in range(4):
                pb = self.ps[cb]
                for k in range(NCH):
                    self.mm(pb.ap[:, :], oT.ap[:, k * 128:(k + 1) * 128], wov[:, k, cb * 512:(cb + 1) * 512], k == 0, k == NCH - 1, [oT, wo], [pb])
                cs_ = slice(cb * 512, (cb + 1) * 512)
                self.tt("dve", tt_.ap[:, cs_], pb.ap[:, :], gtb[kind].ap[:, cs_], ALU.mult, [pb, gtb[kind]], [tt_])
            self.stt("dve", tt_.ap[:, :], x_.ap[:, :], ALPHA, tt_.ap[:, :], ALU.mult, ALU.add, [x_, tt_], [tt_])
            if cut < 2:
                continue
            self.layer_norm(tt_, h_, lngb, lnbb, stats)
            self.dma("sync", h1d[g], h_.ap[:, :], [h_], [self.db("h1", g)], "h1_%d" % b)
            self.tt("pool", tt_.ap[:, :], h_.ap[:, :], opb[kind].ap[:, :], ALU.mult, [h_, opb[kind]], [tt_])
            self.tt("pool", f_.ap.rearrange("t (k m) -> t m k", k=16), tt_.ap.rearrange("t (m k) -> t m k", k=16),
                    shb[kind].ap.rearrange("t (m k) -> t m k", k=16), ALU.add, [tt_, shb[kind]], [f_])
            self.dma("sync", ftm[i], f_.ap[:, :], [f_], [self.db("ftm", i)], "ftm")
            if cut < 3:
                continue
            for q in range(4):
                pb = self.ps[4 + q]
                for j in range(4):
                    k = q * 4 + j
                    self.tr(pb.ap[:, j * 128:(j + 1) * 128], h_.ap[:, k * 128:(k + 1) * 128], self.ident(), [h_, self.consts], [pb], inc=(j == 3))
                for j in range(4):
                    k = q * 4 + j
                    ks = slice(k * 128, (k + 1) * 128)
                    self.act(fT32.ap[:, ks], pb.ap[:, j * 128:(j + 1) * 128], AF.Identity, [pb, self.onepT, self.modT], [fT32],
                             scale=self.ocol(l, kind, 1, k), bias=self.mcol(l, kind, 3, k))
            if cut < 4:
                continue
            pl = self.ps[3]
            wrv = wr.ap.rearrange("p (k n) -> p k n", k=16)
            for k in range(NCH):
                self.mm(pl.ap[:, 0:20], fT32.ap[:, k * 128:(k + 1) * 128], wrv[:, k, :], k == 0, False, [fT32, wr], [pl])
            self.mm(pl.ap[:, 0:20], self.consts.ap[0:1, C_ONE:C_ONE + 128], br.ap[0:1, :], False, True, [self.consts, br], [pl])
            self.cp("dve", lg.ap[:, 0:20], pl.ap[:, 0:20], [pl], [lg])
            if not os.environ.get('NO_ROUTING'):
                self.routing(lg, i)
        S.barrier()

    def layer_norm(self, xin, hout, lngb, lnbb, stats):
        sm = self.small
        for c in range(4):
            self.S.op("dve", lambda e, c=c: e.bn_stats(stats.ap[:, c * 6:(c + 1) * 6], xin.ap[:, c * 512:(c + 1) * 512]), reads=[xin], writes=[stats])
        self.S.op("dve", lambda e: e.bn_aggr(sm.ap[:, 16:18], stats.ap[:, 0:24]), reads=[stats], writes=[sm])
        self.act(sm.ap[:, 18:19], sm.ap[:, 17:18], AF.Ln, [sm], [sm], bias=EPS)
        self.act(sm.ap[:, 18:19], sm.ap[:, 18:19], AF.Exp, [sm], [sm], scale=-0.5)
        self.ts("dve", xin.ap[:, :], xin.ap[:, :], sm.ap[:, 16:17], sm.ap[:, 18:19], ALU.subtract, ALU.mult, [xin, sm], [xin])
        self.tt("pool", xin.ap[:, :], xin.ap[:, :], lngb.ap[:, :], ALU.mult, [xin, lngb], [xin])
        self.tt("dve", hout.ap[:, :], xin.ap[:, :], lnbb.ap[:, :], ALU.add, [xin, lnbb], [hout])

    def routing(self, lg, i):
        sm = self.small
        L = lg.ap
        c = lambda a, b=None: sm.ap[:, a:(b if b is not None else a + 1)]
        R, W = [lg, sm], [sm]
        self.S.op("dve", lambda e: e.reduce_max(out=c(20), in_=L[:, 0:4], axis=AX.X), reads=R, writes=W)
        self.ts("dve", c(21), c(20), -1.0, None, ALU.mult, None, R, W)
        self.act(c(24, 28), L[:, 0:4], AF.Exp, R, W, bias=c(21), accum=c(22))
        self.S.op("dve", lambda e: e.reciprocal(c(23), c(22)), reads=R, writes=W)
        self.ts("dve", c(28, 32), L[:, 0:4], c(20), None, ALU.is_equal, None, R, W)
        self.ts("dve", c(32, 36), L[:, 4:8], c(28), None, ALU.mult, None, R, W)
        for g in range(1, 4):
            self.stt("dve", c(32, 36), L[:, 4 + 4 * g:8 + 4 * g], c(28 + g), c(32, 36), ALU.mult, ALU.add, R, W)
        self.S.op("dve", lambda e: e.reduce_max(out=c(36), in_=c(32, 36), axis=AX.X), reads=R, writes=W)
        self.ts("dve", c(37), c(36), -1.0, None, ALU.mult, None, R, W)
        self.act(c(40, 44), c(32, 36), AF.Exp, R, W, bias=c(37))
        self.ts("dve", c(44, 48), c(32, 36), c(36), None, ALU.is_equal, None, R, W)
        self.stt("dve", c(48, 52), c(44, 48), -1e30, c(32, 36), ALU.mult, ALU.add, R, W)
        self.S.op("dve", lambda e: e.reduce_max(out=c(38), in_=c(48, 52), axis=AX.X), reads=R, writes=W)
        self.ts("dve", c(52, 56), c(48, 52), c(38), None, ALU.is_equal, None, R, W)
        self.tt("dve", c(52, 56), c(52, 56), c(44, 48), ALU.add, R, W)
        self.tt("dve", c(40, 44), c(40, 44), c(52, 56), ALU.mult, R, W)
        self.S.op("dve", lambda e: e.reduce_sum(out=c(39), in_=c(40, 44), axis=AX.X), reads=R, writes=W)
        self.S.op("dve", lambda e: e.reciprocal(c(39), c(39)), reads=R, writes=W)
        self.tt("dve", c(39), c(39), c(23), ALU.mult, R, W)
        self.ts("dve", c(40, 44), c(40, 44), c(39), None, ALU.mult, None, R, W)
        for g in range(4):
            o = i * 16 + g * 4
            self.ts("dve", self.wt_all.ap[:, o:o + 4], c(40, 44), c(28 + g), None, ALU.mult, None, R, [self.wt_all])
            self.ts("dve", self.A_all.ap[:, o:o + 4], c(52, 56), c(28 + g), None, ALU.mult, None, R, [self.A_all])

    def phase_moe(self, l, ntiles, w1, w3, w2):
        S, A = self.S, self.A
        fTd = self.dram["fT"]
        A.reset()
        acc = A.bf16(ntiles * 2048)
        self.moe_acc = acc
        keep = A.off
        W1 = [A.bf16(16 * 256) for _ in range(2)]
        W3 = [A.bf16(16 * 256) for _ in range(2)]
        W2 = [A.bf16(2 * 2048) for _ in range(2)]
        fg = [A.bf16(16 * 512) for _ in range(2)]
        hT = [A.bf16(512) for _ in range(4)]
        su = [A.bf16(512) for _ in range(2)]
        ngrp = ntiles // 4
        half = ntiles * 1024
        self.ms("pool", acc.ap[:, 0:half], 0.0, [acc])
        self.ms("dve", acc.ap[:, half:2 * half], 0.0, [acc])
        n = 0
        ny = 0
        nh = 0
        for e_ in range(16):
            for qt in range(4):
                u = e_ * 4 + qt
                ub = u % 2
                w1v = W1[ub].ap.rearrange("p (k n) -> p k n", k=16)
                w3v = W3[ub].ap.rearrange("p (k n) -> p k n", k=16)
                w2v = W2[ub].ap.rearrange("p (c n) -> p c n", c=2)
                qs = slice(qt * 256, (qt + 1) * 256)
                self.dma("pool", w1v, w1[l, e_].rearrange("(k p) n -> p k n", p=128)[:, :, qs], [], [W1[ub]], "W1_%d" % ub)
                self.dma("pool", w3v, w3[l, e_].rearrange("(k p) n -> p k n", p=128)[:, :, qs], [], [W3[ub]], "W3_%d" % ub)
                self.dma("pool", w2v, w2[l, e_, qs, :].rearrange("(c p) n -> p c n", p=128), [], [W2[ub]], "W2_%d" % ub)
                for gi in range(ngrp):
                    f = fg[n % 2]
                    fv = f.ap.rearrange("p (k n) -> p k n", k=16)
                    self.dma("sync", fv, fTd[gi], [self.db("fT", gi)], [f], "fg%d" % (n % 2))
                    n += 1
                    hs = []
                    for dcq in range(2):
                        pu, pv = self.ps[2 * (nh % 2)], self.ps[2 * (nh % 2) + 1]
                        h_ = hT[nh % 4]
                        s_ = su[nh % 2]
                        nh += 1
                        ds = slice(dcq * 128, (dcq + 1) * 128)
                        for k in range(NCH):
                            self.mm(pu.ap[:, :], w1v[:, k, ds], fv[:, k, :], k == 0, k == NCH - 1, [W1[ub], f], [pu])
                        for k in range(NCH):
                            self.mm(pv.ap[:, :], w3v[:, k, ds], fv[:, k, :], k == 0, k == NCH - 1, [W3[ub], f], [pv])
                        self.act(s_.ap[:, :], pu.ap[:, :], AF.Silu, [pu], [s_])
                        self.tt("dve", h_.ap[:, :], pv.ap[:, :], s_.ap[:, :], ALU.mult, [pv, s_], [h_])
                        hs.append(h_)
                    for t in range(4):
                        i = gi * 4 + t
                        wcol = self.wt_all.ap[:, i * 16 + e_:i * 16 + e_ + 1]
                        for cb in range(4):
                            py = self.ps[4 + ny % 4]
                            ny += 1
                            for dcq in range(2):
                                self.mm(py.ap[:, :], hs[dcq].ap[:, t * 128:(t + 1) * 128], w2v[:, dcq, cb * 512:(cb + 1) * 512], dcq == 0, dcq == 1, [hs[dcq], W2[ub]], [py])
                            asl = acc.ap[:, i * 2048 + cb * 512:i * 2048 + (cb + 1) * 512]
                            self.stt("dve", asl, py.ap[:, :], wcol, asl, ALU.mult, ALU.add, [py, self.wt_all, acc], [acc])
        S.barrier()
        return keep

    def phase_sort(self, ntiles, l=0):
        S, A = self.S, self.A
        ftm = self.dram["ftm"]
        NB = 2 * ntiles * 128 // MOE_B + 16
        Xs = self.dram["Xs"]
        A.reset()
        cnt = A.f32(16)
        nb = A.f32(16)
        tmp = A.f32(16)
        pst = A.f32(16)
        pen = A.f32(16)
        be = A.f32(32)
        dst = A.f32(16)
        dA = A.f32(16)
        eq = A.f32(16)
        col = A.f32(8)
        fb = [A.bf16(2048) for _ in range(2)]
        Aall, wall = self.A_all, self.wt_all
        pc = self.ps[0]
        for i in range(ntiles):
            self.mm(pc.ap[:, 0:16], self.consts.ap[:, C_ONE:C_ONE + 128], Aall.ap[:, i * 16:(i + 1) * 16], i == 0, i == ntiles - 1, [self.consts, Aall], [pc])
        self.cp("dve", cnt.ap[:, :], pc.ap[:, 0:16], [pc], [cnt])
        nmax = ntiles * 128 // MOE_B
        self.ts("dve", nb.ap[:, :], cnt.ap[:, :], 0.0, None, ALU.is_gt, None, [cnt], [nb])
        for m in range(1, nmax):
            self.ts("dve", tmp.ap[:, :], cnt.ap[:, :], float(m * MOE_B), None, ALU.is_gt, None, [cnt], [tmp])
            self.tt("dve", nb.ap[:, :], nb.ap[:, :], tmp.ap[:, :], ALU.add, [nb, tmp], [nb])
        self.ts("dve", nb.ap[:, :], nb.ap[:, :], float(MOE_B), None, ALU.mult, None, [nb], [nb])
        self.ms("dve", pst.ap[:, 0:1], 0.0, [pst])
        for e_ in range(1, 16):
            self.tt("dve", pst.ap[:, e_:e_ + 1], pst.ap[:, e_ - 1:e_], nb.ap[:, e_ - 1:e_], ALU.add, [pst, nb], [pst])
        self.tt("dve", pen.ap[:, :], pst.ap[:, :], nb.ap[:, :], ALU.add, [pst, nb], [pen])
        for b in range(NB):
            self.ts("dve", tmp.ap[:, :], pen.ap[:, :], float(b * MOE_B), None, ALU.is_le, None, [pen], [tmp])
            self.S.op("dve", lambda e, b=b: e.reduce_sum(out=be.ap[:, b:b + 1], in_=tmp.ap[:, :], axis=AX.X), reads=[tmp], writes=[be])
        self.ts("dve", be.ap[:, 0:NB], be.ap[:, 0:NB], 15.0, None, ALU.min, None, [be], [be])
        for b in range(NB):
            for q in range(4):
                self.stt("dve", col.ap[:, 0:1], be.ap[:, b:b + 1], 512.0, self.consts.ap[:, C_P4 + q:C_P4 + q + 1], ALU.mult, ALU.add, [be, self.consts], [col])
                if l:
                    self.ts("dve", col.ap[:, 0:1], col.ap[:, 0:1], float(l * 8192), None, ALU.add, None, [col], [col])
                self.cp("dve", self.widx.ap[:, b * 12 + q:b * 12 + q + 1], col.ap[:, 0:1], [col], [self.widx])
            for c_ in range(8):
                self.stt("dve", col.ap[:, 0:1], be.ap[:, b:b + 1], 1024.0, self.consts.ap[:, C_PC + c_:C_PC + c_ + 1], ALU.mult, ALU.add, [be, self.consts], [col])
                if l:
                    self.ts("dve", col.ap[:, 0:1], col.ap[:, 0:1], float(l * 16384), None, ALU.add, None, [col], [col])
                self.cp("dve", self.widx.ap[:, b * 12 + 4 + c_:b * 12 + 5 + c_], col.ap[:, 0:1], [col], [self.widx])
        for i in range(ntiles):
            pr = self.ps[1 + i % 2]
            for j in range(i):
                self.mm(pr.ap[:, 0:16], self.consts.ap[:, C_ONE:C_ONE + 128], Aall.ap[:, j * 16:(j + 1) * 16], j == 0, False, [self.consts, Aall], [pr])
            self.mm(pr.ap[:, 0:16], self.consts.ap[:, C_SLT:C_SLT + 128], Aall.ap[:, i * 16:(i + 1) * 16], i == 0, True, [self.consts, Aall], [pr])
            Ai = Aall.ap[:, i * 16:(i + 1) * 16]
            wi = wall.ap[:, i * 16:(i + 1) * 16]
            self.tt("dve", dst.ap[:, :], pr.ap[:, 0:16], pst.ap[:, :], ALU.add, [pr, pst], [dst])
            self.tt("dve", dA.ap[:, :], dst.ap[:, :], Ai, ALU.mult, [dst, Aall], [dA])
            self.S.op("dve", lambda e: e.reduce_sum(out=col.ap[:, 1:2], in_=dA.ap[:, :], axis=AX.X), reads=[dA], writes=[col])
            self.S.op("dve", lambda e: e.reduce_max(out=col.ap[:, 2:3], in_=dA.ap[:, :], axis=AX.X), reads=[dA], writes=[col])
            self.tt("dve", col.ap[:, 3:4], col.ap[:, 1:2], col.ap[:, 2:3], ALU.subtract, [col], [col])
            self.cp("dve", self.didx.ap[:, 2 * i:2 * i + 1], col.ap[:, 3:4], [col], [self.didx])
            self.cp("dve", self.didx.ap[:, 2 * i + 1:2 * i + 2], col.ap[:, 2:3], [col], [self.didx])
            self.ts("dve", col.ap[:, 5:7], col.ap[:, 2:4], float(26 * MOE_B), None, ALU.add, None, [col], [col])
            self.cp("dve", self.didx2.ap[:, 2 * i:2 * i + 1], col.ap[:, 6:7], [col], [self.didx2])
            self.cp("dve", self.didx2.ap[:, 2 * i + 1:2 * i + 2], col.ap[:, 5:6], [col], [self.didx2])
            self.ts("dve", eq.ap[:, :], dA.ap[:, :], col.ap[:, 2:3], None, ALU.is_equal, None, [dA, col], [eq])
            self.tt("dve", eq.ap[:, :], eq.ap[:, :], wi, ALU.mult, [eq, wall], [eq])
            self.S.op("dve", lambda e, i=i: e.reduce_sum(out=self.dw.ap[:, 2 * i + 1:2 * i + 2], in_=eq.ap[:, :], axis=AX.X), reads=[eq], writes=[self.dw])
            self.S.op("dve", lambda e, wi=wi: e.reduce_sum(out=col.ap[:, 4:5], in_=wi, axis=AX.X), reads=[wall], writes=[col])
            self.tt("dve", self.dw.ap[:, 2 * i:2 * i + 1], col.ap[:, 4:5], self.dw.ap[:, 2 * i + 1:2 * i + 2], ALU.subtract, [col, self.dw], [self.dw])
            f = fb[i % 2]
            self.dma("sync", f.ap[:, :], ftm[i], [self.db("ftm", i)], [f], "fb%d" % (i % 2))
            for w_ in range(2):
                self.S.op("pool", lambda e, f=f, i=i, w_=w_: e.indirect_dma_start(
                    out=self.dram["big2d"], out_offset=bass.IndirectOffsetOnAxis(ap=self.didx.ap[:, 2 * i + w_:2 * i + w_ + 1], axis=0),
                    in_=f.ap[:, :], in_offset=None), reads=[f, self.didx], writes=[self.db("Xs", 0)], dma="xsc%d" % (i % 2))
        S.barrier()
        return NB

    def phase_moe_sorted(self, l, NB, w1, w3, w2):
        S, A = self.S, self.A
        Xs = self.dram["Xs"]
        Yd = self.dram["Yd"]
        w1v = w1.rearrange("l e (p q r) c -> (l e p q) (r c)", q=4, r=4)
        w3v = w3.rearrange("l e (p q r) c -> (l e p q) (r c)", q=4, r=4)
        w2v = w2.rearrange("l e r c -> (l e r) c")
        A.reset()
        W1 = A.bf16(16 * 1024)
        W3 = A.bf16(16 * 1024)
        W2 = A.bf16(8 * 2048)
        xsb = [A.bf16(2048) for _ in range(4)]
        XT = A.bf16(16 * MOE_B)
        hT = A.bf16(8 * MOE_B)
        su = [A.bf16(MOE_B) for _ in range(2)]
        yst = [A.bf16(2048) for _ in range(2)]
        W1v_ = W1.ap.rearrange("p (k c) -> p k c", k=16)
        W3v_ = W3.ap.rearrange("p (k c) -> p k c", k=16)
        W2v_ = W2.ap.rearrange("p (c n) -> p c n", c=8)
        XTv = XT.ap.rearrange("p (k s) -> p k s", k=16)
        hTv = hT.ap.rearrange("p (c s) -> p c s", c=8)
        ny = 0
        nh = 0
        mcut = int(os.environ.get("MOE_CUT", 99))
        if mcut < 5:
            NB = 1
        for b in range(NB):
            for (Wt, Wv_, src, nm) in ((W1, W1v_, w1v, "gw1"), (W3, W3v_, w3v, "gw3")):
                for q in range(4):
                    self.S.op("pool", lambda e, Wt=Wt, src=src, q=q, b=b: e.indirect_dma_start(
                        out=Wt.ap[:, q * 4096:(q + 1) * 4096], out_offset=None, in_=src,
                        in_offset=bass.IndirectOffsetOnAxis(ap=self.widx.ap[:, b * 12 + q:b * 12 + q + 1], axis=0)),
                        reads=[self.widx], writes=[Wt], dma=nm)
            if mcut < 1:
                continue
            for t in range(4):
                self.dma("sync", xsb[t].ap[:, :], Xs[b * MOE_B + t * 128:b * MOE_B + (t + 1) * 128, :], [self.db("Xs", 0)], [xsb[t]], "xsb%d" % t)
            for t in range(4):
                xv = xsb[t].ap.rearrange("p (k m) -> p k m", k=16)
                for half in range(2):
                    pb = self.ps[6 + half]
                    pbb = pb.ap.bitcast(BF16)
                    for j in range(8):
                        k = half * 8 + j
                        self.tr(pbb[:, j * 128:(j + 1) * 128], xv[:, k, :], self.identb.ap[:, :], [xsb[t], self.identb], [pb], inc=(j == 7))
                    self.cp("act" if half else "dve", XTv[:, half * 8:(half + 1) * 8, t * 128:(t + 1) * 128], pbb.rearrange("p (j s) -> p j s", j=8), [pb], [XT])
            if mcut < 2:
                continue
            for c_ in range(8):
                pu, pv = self.ps[2 * (nh % 2)], self.ps[2 * (nh % 2) + 1]
                s_ = su[nh % 2]
                nh += 1
                ds_ = slice(c_ * 128, (c_ + 1) * 128)
                for k in range(NCH):
                    self.mm(pu.ap[:, :], W1v_[:, k, ds_], XTv[:, k, :], k == 0, k == NCH - 1, [W1, XT], [pu])
                for k in range(NCH):
                    self.mm(pv.ap[:, :], W3v_[:, k, ds_], XTv[:, k, :], k == 0, k == NCH - 1, [W3, XT], [pv])
                self.act(s_.ap[:, :], pu.ap[:, :], AF.Silu, [pu], [s_])
                self.tt("dve", hTv[:, c_, :], pv.ap[:, :], s_.ap[:, :], ALU.mult, [pv, s_], [hT])
            if mcut < 3:
                continue
            for c_ in range(8):
                self.S.op("pool", lambda e, c_=c_, b=b: e.indirect_dma_start(
                    out=W2.ap[:, c_ * 2048:(c_ + 1) * 2048], out_offset=None, in_=w2v,
                    in_offset=bass.IndirectOffsetOnAxis(ap=self.widx.ap[:, b * 12 + 4 + c_:b * 12 + 5 + c_], axis=0)),
                    reads=[self.widx], writes=[W2], dma="gw2")
            if mcut < 4:
                continue
            for t in range(4):
                ys = yst[t % 2]
                for cb in range(4):
                    py = self.ps[4 + ny % 2]
                    ny += 1
                    for c_ in range(8):
                        self.mm(py.ap[:, :], hTv[:, c_, t * 128:(t + 1) * 128], W2v_[:, c_, cb * 512:(cb + 1) * 512], c_ == 0, c_ == 7, [hT, W2], [py])
                    self.cp("act" if cb % 2 else "dve", ys.ap[:, cb * 512:(cb + 1) * 512], py.ap[:, :], [py], [ys])
                self.dma("sync", Yd[b * MOE_B + t * 128:b * MOE_B + (t + 1) * 128, :], ys.ap[:, :], [ys], [self.db("Yd", 0)], "yst%d" % (t % 2))
        S.barrier()

    def phase_ln2(self, l, tiles, keep, lng_in, lnb_in, dst_fn, sorted_=False):
        S, A = self.S, self.A
        h1d = self.dram["h1"]
        modrow = self.dram["modrow"]
        acc = None if sorted_ else self.moe_acc
        A.off = keep
        if sorted_:
            Yd = self.dram["Yd"]
            ylo = [A.bf16(2048) for _ in range(2)]
            yhi = [A.bf16(2048) for _ in range(2)]
        rowbuf = A.f32(2048)
        gtb = [A.f32(2048) for _ in range(2)]
        lngb = A.f32(2048)
        lnbb = A.f32(2048)
        ht = [A.f32(2048) for _ in range(2)]
        tt_ = [A.f32(2048) for _ in range(2)]
        ho = [A.f32(2048) for _ in range(2)]
        stats = A.f32(24)
        kinds = sorted(set(1 if g < NCTX_T else 0 for g in tiles))
        for kind in kinds:
            self.bcast_row(gtb[kind], gtb[kind].ap, modrow[l, kind:kind + 1, 5 * D:6 * D], D, self.db("modrow", l), rowbuf, "rowbuf")
        self.bcast_row(lngb, lngb.ap, lng_in, D, None, rowbuf, "rowbuf")
        self.bcast_row(lnbb, lnbb.ap, lnb_in, D, None, rowbuf, "rowbuf")
        for i, g in enumerate(tiles):
            kind = 1 if g < NCTX_T else 0
            b = i % 2
            self.dma("sync", ht[b].ap[:, :], h1d[g], [self.db("h1", g)], [ht[b]], "ht%d" % b)
            if sorted_:
                for (yb, w_) in ((ylo[b], 0), (yhi[b], 1)):
                    self.S.op("pool", lambda e, yb=yb, i=i, w_=w_: e.indirect_dma_start(
                        out=yb.ap[:, :], out_offset=None, in_=self.dram["big2d"],
                        in_offset=bass.IndirectOffsetOnAxis(ap=self.didx2.ap[:, 2 * i + w_:2 * i + w_ + 1], axis=0)),
                        reads=[self.didx2, self.db("Yd", 0)], writes=[yb], dma="yg%d_%d" % (w_, b))
                self.ts("dve", tt_[b].ap[:, :], ylo[b].ap[:, :], self.dw.ap[:, 2 * i:2 * i + 1], None, ALU.mult, None, [ylo[b], self.dw], [tt_[b]])
                self.stt("dve", tt_[b].ap[:, :], yhi[b].ap[:, :], self.dw.ap[:, 2 * i + 1:2 * i + 2], tt_[b].ap[:, :], ALU.mult, ALU.add, [yhi[b], self.dw, tt_[b]], [tt_[b]])
                self.tt("dve", tt_[b].ap[:, :], tt_[b].ap[:, :], gtb[kind].ap[:, :], ALU.mult, [tt_[b], gtb[kind]], [tt_[b]])
            else:
                self.tt("dve", tt_[b].ap[:, :], acc.ap[:, i * 2048:(i + 1) * 2048], gtb[kind].ap[:, :], ALU.mult, [acc, gtb[kind]], [tt_[b]])
            self.stt("dve", tt_[b].ap[:, :], ht[b].ap[:, :], ALPHA, tt_[b].ap[:, :], ALU.mult, ALU.add, [ht[b], tt_[b]], [tt_[b]])
            self.layer_norm(tt_[b], ho[b], lngb, lnbb, stats)
            dst, dbuf = dst_fn(g)
            self.dma("sync", dst, ho[b].ap[:, :], [ho[b]], [dbuf], "ho%d" % b)
        S.barrier()

    def phase_nat_inproj(self):
        S, A = self.S, self.A
        h2 = self.dram["h2"]
        w_in = self.din("nat_w_in", [D, 3 * D])
        qT = self.dscr("nqT", [NTP, 128, 16, 128], BF16)
        kT = self.dscr("nkT", [16, 128, NTP * 128], BF16)
        va = self.dscr("nva", [NTP, 128, 16, 132], BF16)
        A.reset()
        aT = A.bf16(NTP * 2048)
        xs = [A.f32(D) for _ in range(2)]
        wsl = [A.bf16(16 * 512) for _ in range(2)]
        stg = [A.bf16(512) for _ in range(4)]
        vst = [A.bf16(4 * 132) for _ in range(2)]
        tiles = list(range(NTP))
        self.build_aT(aT, tiles, lambda g: (h2[g], self.db("h2", g)), 1, 0, 0, xs, gm=True)
        for v_ in vst:
            vv = v_.ap.rearrange("p (h c) -> p h c", h=4)
            self.ms("pool", v_.ap[:, :], 0.0, [v_])
            self.ms("pool", vv[:, :, 128:129], 1.0, [v_])
        wv = w_in.rearrange("(k p) n -> p k n", p=128)
        aTv = aT.ap.rearrange("p (g k n) -> p g k n", g=NTP // 4, k=16)
        n = 0
        nv = 0
        qscale = 128.0 ** -0.5
        for sl_ in range(12):
            w = wsl[sl_ % 2]
            wap = w.ap.rearrange("p (k n) -> p k n", k=16)
            self.dma("pool", wap, wv[:, :, sl_ * 512:(sl_ + 1) * 512], [], [w], "nwsl%d" % (sl_ % 2))
            if sl_ < 8:
                for h4 in range(4):
                    hh = (sl_ % 4) * 4 + h4
                    for grp in range(NTP // 4):
                        pb = self.ps[n % 4]
                        st = stg[n % 4]
                        n += 1
                        for k in range(NCH):
                            self.mm(pb.ap[:, :], wap[:, k, h4 * 128:(h4 + 1) * 128], aTv[:, grp, k, :], k == 0, k == NCH - 1, [w, aT], [pb])
                        if sl_ < 4:
                            self.act(st.ap[:, :], pb.ap[:, :], AF.Copy, [pb], [st], scale=qscale)
                            self.dma("sync", qT[grp * 4:grp * 4 + 4, :, hh, :].rearrange("t p q -> p t q"), st.ap.rearrange("p (t q) -> p t q", t=4),
                                     [st], [self.db("nqT", grp)], "nstg%d" % ((n - 1) % 4))
                        else:
                            self.cp("dve", st.ap[:, :], pb.ap[:, :], [pb], [st])
                            self.dma("sync", kT[hh, :, grp * 512:(grp + 1) * 512], st.ap[:, :], [st], [self.db("nkT", 0)], "nstg%d" % ((n - 1) % 4))
            else:
                cb = sl_ - 8
                for i in range(NTP):
                    pb = self.ps[4 + n % 4]
                    n += 1
                    v_ = vst[nv % 2]
                    for k in range(NCH):
                        self.mm(pb.ap[:, :], aTv[:, i // 4, k, (i % 4) * 128:(i % 4 + 1) * 128], wap[:, k, :], k == 0, k == NCH - 1, [w, aT], [pb])
                    vv = v_.ap.rearrange("p (h c) -> p h c", h=4)
                    self.cp("dve" if nv % 2 else "act", vv[:, :, 0:128], pb.ap.rearrange("p (h c) -> p h c", h=4), [pb], [v_])
                    self.dma("sync", va[i, :, cb * 4:(cb + 1) * 4, :], vv, [v_], [self.db("nva", i)], "nvst%d" % (nv % 2))
                    nv += 1
        S.barrier()

    def phase_nat_attn(self):
        S, A = self.S, self.A
        qT, kT, va, og = self.dram["nqT"], self.dram["nkT"], self.dram["nva"], self.dram["og"]
        bias_in = self.din("nat_bias", [3, 16, 128, 5 * 128])
        A.reset()
        qb = [A.bf16(16 * 128) for _ in range(2)]
        kb_ = [A.bf16(16 * 896) for _ in range(2)]
        vb = [A.bf16(7 * 16 * 132) for _ in range(2)]
        ball = A.f32(16 * 640)
        sb_ = [A.f32(640) for _ in range(2)]
        pT = [A.bf16(896) for _ in range(2)]
        ogt = [A.bf16(2048) for _ in range(2)]
        bav = ball.ap.rearrange("p (h n) -> p h n", h=16)

        def loads(j):
            g = j + NCTX_T
            kbr = min(max(2 * j - 4, 0), 26)
            b = j % 2
            q_, k_, v_ = qb[b], kb_[b], vb[b]
            kv = k_.ap.rearrange("p (h t) -> p h t", h=16)
            vv = v_.ap.rearrange("p (c h e) -> p c h e", c=7, h=16)
            self.dma("sync", q_.ap.rearrange("p (h q) -> p h q", h=16), qT[g], [self.db("nqT", g // 4)], [q_], "nq%d" % b)
            t0 = 256 + 64 * kbr
            self.dma("sync", kv[:, :, 0:640], kT[:, :, t0:t0 + 640].rearrange("h p t -> p h t"), [self.db("nkT", 0)], [k_], "nk%d" % b)
            self.dma("sync", kv[:, :, 640:896], kT[:, :, 0:256].rearrange("h p t -> p h t"), [self.db("nkT", 0)], [k_], "nk%d" % b)
            for c in range(7):
                gt = (NCTX_T + kbr // 2 + c) if c < 5 else (c - 5)
                self.dma("sync", vv[:, c, :, :], va[gt], [self.db("nva", gt)], [v_], "nv%d" % b)

        n = 0
        loads(0)
        last_cls = -1
        for j in range(NOWN_T):
            g = j + NCTX_T
            cls = min(j, 2)
            b = j % 2
            q_, k_, v_ = qb[b], kb_[b], vb[b]
            kv = k_.ap.rearrange("p (h t) -> p h t", h=16)
            vv = v_.ap.rearrange("p (c h e) -> p c h e", c=7, h=16)
            if cls != last_cls:
                for hq in range(4):
                    self.dma("sync", bav[:, hq * 4:(hq + 1) * 4, :], bias_in[cls, hq * 4:(hq + 1) * 4].rearrange("h p n -> p h n"), [], [ball], "nball")
                last_cls = cls
            if j + 1 < NOWN_T:
                loads(j + 1)
            o_ = ogt[b]
            for h in range(16):
                hb = n % 2
                n += 1
                pS0, pS1, pO = self.ps[3 * hb], self.ps[3 * hb + 1], self.ps[3 * hb + 2]
                sc_, p_ = sb_[hb], pT[hb]
                for c in range(7):
                    pd = pS0 if c < 4 else pS1
                    self.mm(pd.ap[:, (c % 4) * 128:(c % 4 + 1) * 128], kv[:, h, c * 128:(c + 1) * 128], q_.ap[:, h * 128:(h + 1) * 128], True, True, [k_, q_], [pd])
                self.tt("dve", sc_.ap[:, 0:512], pS0.ap[:, :], bav[:, h, 0:512], ALU.add, [pS0, ball], [sc_])
                self.tt("dve", sc_.ap[:, 512:640], pS1.ap[:, 0:128], bav[:, h, 512:640], ALU.add, [pS1, ball], [sc_])
                self.act(p_.ap[:, 640:896], pS1.ap[:, 128:384], AF.Exp, [pS1], [p_])
                self.act(p_.ap[:, 0:640], sc_.ap[:, :], AF.Exp, [sc_], [p_])
                for c in range(7):
                    self.mm(pO.ap[:, 0:129], p_.ap[:, c * 128:(c + 1) * 128], vv[:, c, h, 0:129], c == 0, c == 6, [p_, v_], [pO])
                rc = self.small.ap[:, 60 + hb:61 + hb]
                self.S.op("dve", lambda e, rc=rc, pO=pO: e.reciprocal(rc, pO.ap[:, 128:129]), reads=[pO], writes=[self.small])
                self.ts("dve", o_.ap[:, h * 128:(h + 1) * 128], pO.ap[:, 0:128], rc, None, ALU.mult, None, [pO, self.small], [o_])
            self.dma("sync", og[g], o_.ap[:, :], [o_], [self.db("og", g)], "nog%d" % b)
        S.barrier()

    def finish(self, out_waits):
        for t in out_waits:
            tok = t.buf.w
            if tok is not None:
                k, v = tok
                if self.S.waited["sync"].get(k, 0) < v:
                    self.S.streams["sync"].append(("w", k, v))
                    self.S.waited["sync"][k] = v
        self.S.barrier()
        self.S.emit(self.nc)
        return self.nc


LASTP = None
SORTED = True


def build(debug=None, stop=None):
    global LASTP
    P = Prog(debug)
    LASTP = P
    P.setup()
    P.phase_mod()
    if stop == "mod":
        return P.finish([])
    P.phase_gla_inproj()
    if stop == "inproj":
        return P.finish([])
    keep = P.phase_gla_prep()
    if stop == "prep":
        return P.finish([])
    P.phase_gla_scan(keep)
    if stop == "scan":
        return P.finish([])
    x, ctx = P.dram["x"], P.dram["ctx"]
    ln_g = P.din("ln_g", [2, 2, D])
    ln_b = P.din("ln_b", [2, 2, D])
    wr = P.din("moe_wr", [2, D, 20])
    br = P.din("moe_br", [2, 20])
    w1 = P.din("moe_w1", [2, 16, D, 1024])
    w3 = P.din("moe_w3", [2, 16, D, 1024])
    w2 = P.din("moe_w2", [2, 16, 1024, D])
    gla_w_out = P.din("gla_w_out", [D, D])

    def xsrc0(g):
        if g < NCTX_T:
            return ctx[g * 128:(g + 1) * 128, :], None
        return x[(g - NCTX_T) * 128:(g - NCTX_T + 1) * 128, :], None

    tiles0 = list(range(NTP))
    P.phase_outproj(0, gla_w_out, tiles0, xsrc0, ln_g[0, 0:1, :], ln_b[0, 0:1, :], wr[0], br[0:1, :])
    if stop == "outproj0":
        return P.finish([])
    h2 = P.dscr("h2", [NTP, 128, D])
    if SORTED:
        NB = P.phase_sort(NTP)
        if stop == "sort0":
            return P.finish([])
        P.phase_moe_sorted(0, NB, w1, w3, w2)
        if stop == "moe0":
            return P.finish([])
        P.A.reset()
        P.phase_ln2(0, tiles0, 0, ln_g[0, 1:2, :], ln_b[0, 1:2, :], lambda g: (h2[g], P.db("h2", g)), sorted_=True)
    else:
        keep = P.phase_moe(0, NTP, w1, w3, w2)
        P.phase_ln2(0, tiles0, keep, ln_g[0, 1:2, :], ln_b[0, 1:2, :], lambda g: (h2[g], P.db("h2", g)))
    if stop == "l0":
        return P.finish([])
    P.phase_nat_inproj()
    P.phase_nat_attn()
    if stop == "nat":
        return P.finish([])
    nat_w_out = P.din("nat_w_out", [D, D])
    own = list(range(NCTX_T, NCTX_T + NOWN_T))
    P.phase_outproj(1, nat_w_out, own, lambda g: (h2[g], P.db("h2", g)), ln_g[1, 0:1, :], ln_b[1, 0:1, :], wr[1], br[1:2, :])
    if stop == "outproj1":
        return P.finish([])
    out = P.dout("out", [NOWN_T * 128, D])
    outb = Tn(None)
    if SORTED:
        NB = P.phase_sort(NOWN_T, 1)
        P.phase_moe_sorted(1, NB, w1, w3, w2)
        P.A.reset()
        P.phase_ln2(1, own, 0, ln_g[1, 1:2, :], ln_b[1, 1:2, :], lambda g: (out[(g - NCTX_T) * 128:(g - NCTX_T + 1) * 128, :], outb), sorted_=True)
    else:
        keep = P.phase_moe(1, NOWN_T, w1, w3, w2)
        P.phase_ln2(1, own, keep, ln_g[1, 1:2, :], ln_b[1, 1:2, :], lambda g: (out[(g - NCTX_T) * 128:(g - NCTX_T + 1) * 128, :], outb))
    return P.finish([outb])


def rope_tables(flip):
    t = np.arange(NLAT_T * 128)
    tt = (NLAT_T * 128 - 1 - t) if flip else t
    freqs = (10000.0 ** (-np.arange(64, dtype=np.float32) / 64)).astype(np.float32)
    ang_r = (tt // 64).astype(np.float32)[:, None] * freqs
    ang_c = (tt % 64).astype(np.float32)[:, None] * freqs
    cos = np.ones((NT0 * 128, 8, 64), np.float32)
    sin = np.zeros((NT0 * 128, 8, 64), np.float32)
    for h in range(4):
        cos[256:, 2 * h] = np.cos(ang_r)
        sin[256:, 2 * h] = np.sin(ang_r)
        cos[256:, 2 * h + 1] = np.cos(ang_c)
        sin[256:, 2 * h + 1] = np.sin(ang_c)
    return cos.reshape(NT0, 128, 512), sin.reshape(NT0, 128, 512)


def nat_bias_table(rpb, flip):
    out = np.empty((3, 16, 128, 5, 128), np.float32)
    for cls in range(3):
        j = cls
        kb = min(max(2 * j - 4, 0), 26)
        q = np.arange(128)
        qr_p, qc_p = 2 * j + q // 64, q % 64
        kk = np.arange(640)
        kr_p, kc_p = kb + kk // 64, kk % 64
        if flip:
            qr, qc, kr, kc = 63 - qr_p, 63 - qc_p, 63 - kr_p, 63 - kc_p
        else:
            qr, qc, kr, kc = qr_p, qc_p, kr_p, kc_p
        rs = np.clip(qr - 4, 0, 56)[None, :]
        cs = np.clip(qc - 8, 0, 48)[None, :]
        KR, KC = kr[:, None], kc[:, None]
        valid = (KR >= rs) & (KR < rs + 8) & (KC >= cs) & (KC < cs + 16)
        assert (valid.sum(0) == 128).all()
        dr = np.clip(KR - qr[None, :] + 7, 0, 14)
        dc = np.clip(KC - qc[None, :] + 15, 0, 30)
        for h in range(16):
            bias = np.where(valid, rpb[h][dr, dc], np.float32(NEG)).astype(np.float32)
            out[cls, h] = bias.reshape(5, 128, 128).transpose(1, 0, 2)
    return out.reshape(3, 16, 128, 640)


def host_inputs(inp):
    consts = make_consts()
    ropes = [rope_tables(False), rope_tables(True)]
    wr = np.ascontiguousarray(np.concatenate([inp["moe_w_group"], inp["moe_w_expert"]], axis=2))
    br = np.ascontiguousarray(np.concatenate([inp["moe_b_group"], inp["moe_b_expert"]], axis=1))
    nbias = [nat_bias_table(inp["nat_rpb"][0], False), nat_bias_table(inp["nat_rpb"][0], True)]
    maps = []
    for core in range(8):
        b, hf = core // 2, core % 2
        x = inp["x"][b]
        ctx = inp["ctx"][b]
        w_in = inp["gla_w_in"][0]
        wg = np.concatenate([inp["gla_w_gate"][0], inp["gla_b_gate"][0][:, None, :]], axis=1)
        if hf:
            x = x[::-1]
            ctx = ctx[::-1]
            w_in = np.concatenate([w_in[:, :6144], w_in[:, 6160:6176], w_in[:, 6144:6160]], axis=1)
            wg = wg[::-1]
        m = {
            "consts_in": consts,
            "cc": np.ascontiguousarray(np.stack([inp["c"][b], inp["c_ctx"]], 0)),
            "ada_w": inp["ada_w"], "ada_b": inp["ada_b"],
            "x": np.ascontiguousarray(x), "ctx": np.ascontiguousarray(ctx),
            "gla_w_in": np.ascontiguousarray(w_in),
            "gla_wg": np.ascontiguousarray(wg),
            "rope_cos": ropes[hf][0], "rope_sin": ropes[hf][1],
            "gla_norm_g": np.ascontiguousarray(inp["gla_norm_g"][0][None, :]),
            "gla_w_out": inp["gla_w_out"][0],
            "ln_g": inp["ln_g"], "ln_b": inp["ln_b"],
            "moe_wr": wr, "moe_br": br,
            "moe_w1": inp["moe_w1"], "moe_w3": inp["moe_w3"], "moe_w2": inp["moe_w2"],
            "nat_w_in": inp["nat_w_in"][0], "nat_w_out": inp["nat_w_out"][0], "nat_bias": nbias[hf],
        }
        maps.append(m)
    return maps


_NC = None


def kernel(**inputs):
    global _NC
    inp = {k: np.asarray(v) for k, v in inputs.items()}
    if _NC is None:
        _NC = build()
    nc = _NC
    maps = host_inputs(inp)
    names = set(LASTP.dram.keys())
    maps = [{k: np.ascontiguousarray(v, dtype=np.float32) for k, v in m.items() if k in names} for m in maps]
    res = run_bass_kernel_spmd(nc, maps, core_ids=list(range(8)))
    out = np.empty((4, 4096, D), np.float32)
    for core in range(8):
        b, hf = core // 2, core % 2
        o = res.results[core]["out"]
        if hf:
            out[b, 2048:] = o[::-1]
        else:
            out[b, :2048] = o
    return out
```

```python
import os
import numpy as np
import ml_dtypes
from contextlib import ExitStack
import concourse.bass as bass
import concourse.mybir as mybir
from concourse.bass_utils import run_bass_kernel_spmd

F32 = mybir.dt.float32
BF16 = mybir.dt.bfloat16
AF = mybir.ActivationFunctionType
ALU = mybir.AluOpType
AX = mybir.AxisListType

D = 2048
NCH = 16
NCTX_T = 2
NLAT_T = 32
NP_T = 18
NOWN_T = 16
NT0 = NCTX_T + NLAT_T
NTP = NCTX_T + NP_T
ALPHA = 4.0 ** 0.25
EPS = 1e-5
GLA_IN = 6176
NEG = -30000.0

ENGS = ["sync", "act", "pool", "pe", "dve"]


class Buf:
    __slots__ = ("w", "r")

    def __init__(self):
        self.w = None
        self.r = {}


class Tn:
    __slots__ = ("ap", "buf")

    def __init__(self, ap, buf=None):
        self.ap = ap
        self.buf = buf if buf is not None else Buf()

    def v(self, ap):
        return Tn(ap, self.buf)


class Sched:
    def __init__(self):
        self.streams = {e: [] for e in ENGS}
        self.count = {}
        self.waited = {e: {} for e in ENGS}

    def op(self, eng, fn, reads=(), writes=(), inc=True, dma=None):
        deps = {}

        def need(tok):
            if tok is not None:
                k, v = tok
                if deps.get(k, 0) < v:
                    deps[k] = v

        for t in reads:
            need(t.buf.w)
        for t in writes:
            need(t.buf.w)
            for k, v in t.buf.r.items():
                need((k, v))
        st = self.streams[eng]
        wd = self.waited[eng]
        for k, v in deps.items():
            if eng == "pe" and k == "c_pe":
                continue
            if wd.get(k, 0) >= v:
                continue
            st.append(("w", k, v))
            wd[k] = v
        if dma is not None:
            key = "d_" + dma
            self.count[key] = self.count.get(key, 0) + 16
            tok = (key, self.count[key])
            st.append(("o", fn, key, 16))
        else:
            key = "c_" + eng
            if inc:
                self.count[key] = self.count.get(key, 0) + 1
                tok = (key, self.count[key])
                st.append(("o", fn, key, 1))
            else:
                tok = (key, self.count.get(key, 0) + 1)
                st.append(("o", fn, None, 0))
        for t in reads:
            k, v = tok
            if t.buf.r.get(k, 0) < v:
                t.buf.r[k] = v
        for t in writes:
            t.buf.w = tok
            t.buf.r = {}
        return tok

    def barrier(self):
        for e in ENGS:
            wd = self.waited[e]
            for k, v in self.count.items():
                if e == "pe" and k == "c_pe":
                    continue
                if wd.get(k, 0) < v:
                    self.streams[e].append(("w", k, v))
                    wd[k] = v

    def emit(self, nc):
        with ExitStack() as es:
            sems = {k: es.enter_context(nc.semaphore(k)) for k in self.count}
            block = es.enter_context(nc.Block())

            def runner(name):
                def f(eng):
                    for it in self.streams[name]:
                        if it[0] == "w":
                            eng.wait_ge(sems[it[1]], it[2])
                        else:
                            ins = it[1](eng)
                            if it[2] is not None:
                                ins.then_inc(sems[it[2]], it[3])
                return f

            block.sync(runner("sync"))
            block.scalar(runner("act"))
            block.gpsimd(runner("pool"))
            block.tensor(runner("pe"))
            block.vector(runner("dve"))


class Arena:
    def __init__(self, t, n):
        self.t = t
        self.n = n
        self.off = 0

    def reset(self):
        self.off = 0

    def f32(self, cols):
        a = self.off
        self.off += cols
        assert self.off <= self.n, ("arena overflow", self.off, self.n)
        return Tn(self.t[:, a:a + cols])

    def bf16(self, cols):
        c = (cols + 1) // 2
        a = self.off
        self.off += c
        assert self.off <= self.n, ("arena overflow", self.off, self.n)
        return Tn(self.t[:, a:a + c].bitcast(BF16))


C_ID = 0
C_LA = 128
C_RA = 256
C_LB = 384
C_RB = 512
C_MA = 640
C_MB = 768
C_NEG = 896
C_ONE = 897
C_SLT = 1025
C_P4 = 1153
C_PC = 1157
NCONST = 1165
MOE_B = 512
I32 = mybir.dt.int32


def make_consts():
    c = np.zeros((128, NCONST), np.float32)
    i = np.arange(128)
    tp, t = i[:, None], i[None, :]
    c[:, C_ID:C_ID + 128] = (tp == t)
    c[:, C_LA:C_LA + 128] = (tp <= t) * (-1.0 / 16)
    c[:, C_RA:C_RA + 128] = (tp > t) * (-1.0 / 16)
    c[:, C_LB:C_LB + 128] = (tp >= t) * (-1.0 / 16)
    c[:, C_RB:C_RB + 128] = (tp < t) * (-1.0 / 16)
    c[:, C_MA:C_MA + 128] = (tp <= t)
    c[:, C_MB:C_MB + 128] = (tp >= t)
    c[:, C_NEG] = -1.0 / 16
    c[:, C_ONE:C_ONE + 128] = 1.0
    c[:, C_SLT:C_SLT + 128] = (tp < t)
    for q in range(4):
        c[:, C_P4 + q] = 4 * i + q
    for cc in range(8):
        c[:, C_PC + cc] = 128 * cc + i
    return c


class Prog:
    def __init__(self, debug=None):
        self.debug = debug
        self.nc = bass.Bass("TRN2", target_bir_lowering=False)
        self.S = Sched()
        self.es = ExitStack()
        self.dram = {}
        self.dbufs = {}
        self.outs = []
        self._bc = 0

    def din(self, name, shape, dt=F32):
        h = self.nc.dram_tensor(name, list(shape), dt, kind="ExternalInput")
        self.dram[name] = h.ap()
        return self.dram[name]

    def dout(self, name, shape, dt=F32):
        h = self.nc.dram_tensor(name, list(shape), dt, kind="ExternalOutput")
        self.dram[name] = h.ap()
        return self.dram[name]

    def dscr(self, name, shape, dt=F32):
        if self.debug and name in self.debug:
            return self.dout(name, shape, dt)
        h = self.nc.dram_tensor(name, list(shape), dt)
        self.dram[name] = h.ap()
        return self.dram[name]

    def db(self, name, idx=0):
        k = (name, idx)
        if k not in self.dbufs:
            self.dbufs[k] = Tn(None)
        return self.dbufs[k]

    def dma(self, eng, out_ap, in_ap, reads, writes, tag):
        self.S.op(eng, lambda e: e.dma_start(out=out_ap, in_=in_ap), reads=reads, writes=writes, dma=tag)

    def mm(self, out, lhsT, rhs, start, stop, reads, writes):
        self.S.op("pe", lambda e: e.matmul(out, lhsT, rhs, start=start, stop=stop), reads=reads, writes=writes, inc=stop)

    def tr(self, out, in_, ident, reads, writes, inc=True):
        self.S.op("pe", lambda e: e.transpose(out, in_, ident), reads=reads, writes=writes, inc=inc)

    def tt(self, eng, out, in0, in1, op, reads, writes):
        self.S.op(eng, lambda e: e.tensor_tensor(out=out, in0=in0, in1=in1, op=op), reads=reads, writes=writes)

    def cp(self, eng, out, in_, reads, writes):
        if eng == "act":
            self.S.op(eng, lambda e: e.copy(out, in_), reads=reads, writes=writes)
        else:
            self.S.op(eng, lambda e: e.tensor_copy(out, in_), reads=reads, writes=writes)

    def act(self, out, in_, func, reads, writes, scale=None, bias=None, accum=None):
        kw = {}
        if scale is not None:
            kw["scale"] = scale
        if bias is not None:
            kw["bias"] = bias
        if accum is not None:
            kw["accum_out"] = accum
        self.S.op("act", lambda e: e.activation(out=out, in_=in_, func=func, **kw), reads=reads, writes=writes)

    def stt(self, eng, out, in0, scalar, in1, op0, op1, reads, writes):
        self.S.op(eng, lambda e: e.scalar_tensor_tensor(out=out, in0=in0, scalar=scalar, in1=in1, op0=op0, op1=op1), reads=reads, writes=writes)

    def ts(self, eng, out, in0, s1, s2, op0, op1, reads, writes):
        if s2 is None:
            self.S.op(eng, lambda e: e.tensor_scalar(out=out, in0=in0, scalar1=s1, scalar2=None, op0=op0), reads=reads, writes=writes)
        else:
            self.S.op(eng, lambda e: e.tensor_scalar(out=out, in0=in0, scalar1=s1, scalar2=s2, op0=op0, op1=op1), reads=reads, writes=writes)

    def ms(self, eng, ap, val, writes):
        self.S.op(eng, lambda e: e.memset(ap, val), writes=writes)

    def sb(self, shape, dt, name):
        return self.es.enter_context(self.nc.sbuf_tensor(name, list(shape), dt))

    def setup(self):
        nc = self.nc
        self.arena_t = self.sb([128, 50000], F32, "arena")
        self.A = Arena(self.arena_t, 50000)
        self.consts = Tn(self.sb([128, NCONST], F32, "consts"))
        self.identb = Tn(self.sb([128, 128], BF16, "identb"))
        self.modT = Tn(self.sb([128, 2 * 2 * 96], F32, "modT"))
        self.onepT = Tn(self.sb([128, 2 * 2 * 2 * 16], F32, "onepT"))
        self.wt_all = Tn(self.sb([128, NTP * 16], F32, "wt_all"))
        self.A_all = Tn(self.sb([128, NTP * 16], F32, "A_all"))
        self.didx = Tn(self.sb([128, NTP * 2], I32, "didx"))
        self.didx2 = Tn(self.sb([128, NTP * 2], I32, "didx2"))
        self.dw = Tn(self.sb([128, NTP * 2], F32, "dw"))
        self.widx = Tn(self.sb([128, 26 * 12], I32, "widx"))
        self.small = Tn(self.sb([128, 64], F32, "small"))
        self.ps = [Tn(self.es.enter_context(nc.psum_tensor("ps%d" % i, [128, 512], F32))) for i in range(8)]
        cin = self.din("consts_in", [128, NCONST])
        self.dma("sync", self.consts.ap[:, :], cin[:, :], [], [self.consts], "consts")
        self.S.op("dve", lambda e: e.tensor_copy(self.identb.ap[:, :], self.consts.ap[:, C_ID:C_ID + 128]),
                  reads=[self.consts], writes=[self.identb])

    def ident(self, n=128):
        return self.consts.ap[0:n, C_ID:C_ID + n]

    def phase_mod(self):
        S, A = self.S, self.A
        cc = self.din("cc", [2, D])
        ada_w = self.din("ada_w", [2, D, 6 * D])
        ada_b = self.din("ada_b", [2, 6 * D])
        modrow = self.dscr("modrow", [2, 2, 6 * D])
        A.reset()
        cct = A.f32(D)
        sct = A.f32(D)
        scT = A.bf16(32)
        adab = A.f32(6 * D)
        mrow = A.f32(6 * D)
        wsl = [A.bf16(16 * 512) for _ in range(2)]
        self.dma("sync", cct.ap[0:2, :], cc[:, :], [], [cct], "cc")
        S.op("act", lambda e: e.activation(out=sct.ap[0:2, :], in_=cct.ap[0:2, :], func=AF.Silu), reads=[cct], writes=[sct])
        p0 = self.ps[0]
        for k in range(NCH):
            self.tr(p0.ap[:, 2 * k:2 * k + 2], sct.ap[0:2, k * 128:(k + 1) * 128], self.ident(2), [sct, self.consts], [p0], inc=(k == NCH - 1))
        S.op("dve", lambda e: e.tensor_copy(scT.ap[:, 0:32], p0.ap[:, 0:32]), reads=[p0], writes=[scT])
        n = 0
        for l in range(2):
            for r in range(2):
                self.dma("sync", adab.ap[r:r + 1, :], ada_b[l:l + 1, :], [], [adab], "adab")
            wv = ada_w[l].rearrange("(k p) n -> p k n", p=128)
            for s in range(24):
                w = wsl[n % 2]
                pb = self.ps[1 + n % 2]
                n += 1
                wap = w.ap.rearrange("p (k n) -> p k n", k=16)
                self.dma("pool", wap, wv[:, :, s * 512:(s + 1) * 512], [], [w], "wsl%d" % (n % 2))
                for k in range(NCH):
                    self.mm(pb.ap[0:2, :], scT.ap[:, 2 * k:2 * k + 2], wap[:, k, :], k == 0, k == NCH - 1, [scT, w], [pb])
                sl = slice(s * 512, (s + 1) * 512)
                S.op("dve", lambda e, pb=pb, sl=sl: e.tensor_tensor(out=mrow.ap[0:2, sl], in0=pb.ap[0:2, :], in1=adab.ap[0:2, sl], op=ALU.add),
                     reads=[pb, adab], writes=[mrow])
            self.dma("sync", modrow[l], mrow.ap[0:2, :], [mrow], [self.db("modrow", l)], "mrow")
        mr = A.f32(128)
        for l in range(2):
            for kind in range(2):
                self.dma("sync", mr.ap[0:96, :], modrow[l, kind].rearrange("(c p) -> c p", p=128), [self.db("modrow", l)], [mr], "mr")
                pb = self.ps[3]
                self.tr(pb.ap[:, 0:96], mr.ap[0:96, :], self.ident(96), [mr, self.consts], [pb])
                o = (l * 2 + kind) * 96
                S.op("dve", lambda e, o=o, pb=pb: e.tensor_copy(self.modT.ap[:, o:o + 96], pb.ap[:, 0:96]), reads=[pb], writes=[self.modT])
                for wi, v in enumerate((1, 4)):
                    oo = ((l * 2 + kind) * 2 + wi) * 16
                    S.op("dve", lambda e, o=o, oo=oo, v=v: e.tensor_scalar_add(self.onepT.ap[:, oo:oo + 16], self.modT.ap[:, o + v * 16:o + v * 16 + 16], 1.0),
                         reads=[self.modT], writes=[self.onepT])
        S.barrier()

    def mcol(self, l, kind, v, k):
        o = (l * 2 + kind) * 96 + v * 16 + k
        return self.modT.ap[:, o:o + 1]

    def ocol(self, l, kind, wi, k):
        o = ((l * 2 + kind) * 2 + wi) * 16 + k
        return self.onepT.ap[:, o:o + 1]

    def build_aT(self, aT, tiles, src_fn, l, wi, vsh, xs, gm=False):
        S = self.S
        def ld(i):
            src, dep = src_fn(tiles[i])
            self.dma("sync", xs[i % 2].ap[:, :], src, [dep] if dep is not None else [], [xs[i % 2]], "xs%d" % (i % 2))

        ld(0)
        for i, g in enumerate(tiles):
            kind = 1 if g < NCTX_T else 0
            x = xs[i % 2]
            if i + 1 < len(tiles):
                ld(i + 1)
            for q in range(4):
                pb = self.ps[4 + (i * 4 + q) % 4]
                for j in range(4):
                    k = q * 4 + j
                    self.tr(pb.ap[:, j * 128:(j + 1) * 128], x.ap[:, k * 128:(k + 1) * 128], self.ident(), [x, self.consts], [pb], inc=(j == 3))
                for j in range(4):
                    k = q * 4 + j
                    o = (((i // 4) * 16 + k) * 512 + (i % 4) * 128) if gm else (i * 16 + k) * 128
                    S.op("act", lambda e, pb=pb, j=j, o=o, kind=kind, k=k: e.activation(
                        out=aT.ap[:, o:o + 128], in_=pb.ap[:, j * 128:(j + 1) * 128], func=AF.Identity,
                        scale=self.ocol(l, kind, wi, k), bias=self.mcol(l, kind, vsh, k)),
                        reads=[pb, self.onepT, self.modT], writes=[aT])

    def proj_tm(self, aT, ntiles, w_dram, col0, ncols, wsl, sink, skip=None):
        wv = w_dram.rearrange("(k p) n -> p k n", p=128)
        nb = (ncols + 511) // 512
        n = 0
        for cb in range(nb):
            c0 = col0 + cb * 512
            cw = min(512, col0 + ncols - c0)
            w = wsl[cb % 2]
            wap = w.ap.rearrange("p (k n) -> p k n", k=16)
            self.dma("pool", wap[:, :, 0:cw], wv[:, :, c0:c0 + cw], [], [w], "wsl%d" % (cb % 2))
            for i in range(ntiles):
                if skip is not None and skip(i, cb):
                    continue
                pb = self.ps[n % 4]
                n += 1
                for k in range(NCH):
                    o = (i * 16 + k) * 128
                    self.mm(pb.ap[:, 0:cw], aT.ap[:, o:o + 128], wap[:, k, 0:cw], k == 0, k == NCH - 1, [aT, w], [pb])
                sink(i, cb, c0, cw, pb)

    def phase_gla_inproj(self):
        S, A = self.S, self.A
        x = self.din("x", [NLAT_T * 128, D])
        ctx = self.din("ctx", [NCTX_T * 128, D])
        w_in = self.din("gla_w_in", [D, GLA_IN])
        big = self.dscr("big", [2 * 26 * MOE_B * 2048], BF16)
        proj = big[0:NT0 * 128 * GLA_IN * 2].bitcast(F32).rearrange("(a p n) -> a p n", a=NT0, p=128)
        self.dram["proj"] = proj
        big2d = big.rearrange("(r c) -> r c", c=2048)
        self.dram["big2d"] = big2d
        self.dram["Xs"] = big2d[0:26 * MOE_B, :]
        self.dram["Yd"] = big2d[26 * MOE_B:2 * 26 * MOE_B, :]

        def src(g):
            if g < NCTX_T:
                return ctx[g * 128:(g + 1) * 128, :], None
            return x[(g - NCTX_T) * 128:(g - NCTX_T + 1) * 128, :], None

        for grp in range(2):
            tiles = list(range(grp * 17, grp * 17 + 17))
            A.reset()
            aT = A.bf16(17 * 16 * 128)
            xs = [A.f32(D) for _ in range(2)]
            self.build_aT(aT, tiles, src, 0, 0, 0, xs)
            wsl = [A.bf16(16 * 512) for _ in range(2)]
            stg = [A.f32(512) for _ in range(4)]
            cnt = [0]

            def sink(i, cb, c0, cw, pb):
                n = cnt[0]
                cnt[0] += 1
                st = stg[n % 4]
                if n % 2 == 0:
                    S.op("dve", lambda e: e.tensor_copy(st.ap[:, 0:cw], pb.ap[:, 0:cw]), reads=[pb], writes=[st])
                else:
                    S.op("act", lambda e: e.copy(st.ap[:, 0:cw], pb.ap[:, 0:cw]), reads=[pb], writes=[st])
                g = tiles[i]
                self.dma("sync", proj[g, :, c0:c0 + cw], st.ap[:, 0:cw], [st], [self.db("proj", g)], "stg%d" % (n % 4))

            self.proj_tm(aT, 17, w_in, 0, GLA_IN, wsl, sink, skip=lambda i, cb: tiles[i] >= NTP and (cb < 2 or 8 <= cb < 12))
            S.barrier()

    def bcast_row(self, dst, dst_ap, src_ap, n, dep, rowbuf, tag):
        S = self.S
        self.dma("sync", rowbuf.ap[0:1, 0:n], src_ap, [dep] if dep is not None else [], [rowbuf], tag)
        for c0 in range(0, n, 512):
            cw = min(512, n - c0)
            pb = self.ps[self._bc % 2]
            self._bc += 1
            self.mm(pb.ap[:, 0:cw], self.consts.ap[0:1, C_ONE:C_ONE + 128], rowbuf.ap[0:1, c0:c0 + cw], True, True, [self.consts, rowbuf], [pb])
            S.op("dve", lambda e, pb=pb, c0=c0, cw=cw: e.tensor_copy(dst_ap[:, c0:c0 + cw], pb.ap[:, 0:cw]), reads=[pb], writes=[dst])

    def phase_gla_prep(self):
        import math
        S, A = self.S, self.A
        proj = self.dram["proj"]
        wg_in = self.din("gla_wg", [2, 17, 1024])
        cos_in = self.din("rope_cos", [NT0, 128, 512])
        sin_in = self.din("rope_sin", [NT0, 128, 512])
        qdT = self.dscr("qdT", [2, NT0, 128, 1024], BF16)
        kiT = self.dscr("kiT", [2, NT0, 128, 1024], BF16)
        ke = self.dscr("ke", [2, NT0, 128, 1024], BF16)
        vsc = self.dscr("vsc", [NT0, 128, 2048], BF16)
        sr = self.dscr("sr", [NTP, 128, 2048], BF16)
        A.reset()
        self.decay = A.f32(2 * NT0 * 8)
        arena_keep = A.off
        wg = A.f32(2 * 1024)
        pj = [A.f32(GLA_IN) for _ in range(2)]
        cs = [A.f32(512) for _ in range(2)]
        sn = [A.f32(512) for _ in range(2)]
        qr = A.f32(1024)
        kr = A.f32(1024)
        t1 = A.f32(512)
        t2 = A.f32(512)
        t3 = A.f32(512)
        t4 = A.f32(512)
        glT = [A.f32(128) for _ in range(2)]
        ex = A.f32(1024)
        sp = A.f32(1024)
        E = [A.f32(1024) for _ in range(3)]
        ob = [[A.bf16(1024) for _ in range(3)] for _ in range(2)]
        tT = [[A.bf16(1024) for _ in range(2)] for _ in range(2)]
        vb = [A.bf16(2048) for _ in range(2)]
        rb = [A.bf16(2048) for _ in range(2)]
        self.dma("sync", wg.ap[0:17, :].rearrange("p (d n) -> p d n", d=2), wg_in.rearrange("d p n -> p d n"), [], [wg], "wg")
        for d in range(2):
            self.ms("pool", glT[d].ap[0:17, :], 1.0, [glT[d]])
        psz = [self.ps[0], self.ps[1]]
        pcum = [self.ps[2], self.ps[3]]
        prem = [self.ps[4], self.ps[5]]
        ptr = self.ps[6]
        ptot = self.ps[7]
        ptr_b = ptr.ap.bitcast(BF16)
        lnsc = math.log(1.0 / 16.0)
        def pld(g):
            self.dma("sync", pj[g % 2].ap[:, :], proj[g], [self.db("proj", g)], [pj[g % 2]], "pj%d" % (g % 2))
            self.dma("sync", cs[g % 2].ap[:, :], cos_in[g], [], [cs[g % 2]], "cs%d" % (g % 2))
            self.dma("sync", sn[g % 2].ap[:, :], sin_in[g], [], [sn[g % 2]], "sn%d" % (g % 2))

        pld(0)
        for g in range(NT0):
            p = pj[g % 2]
            c_, s_ = cs[g % 2], sn[g % 2]
            if g + 1 < NT0:
                pld(g + 1)
            cv = c_.ap.rearrange("p (a f) -> p a f", a=8)
            sv = s_.ap.rearrange("p (a f) -> p a f", a=8)
            full = g < NTP
            for (eng, src0, dstt, ta, tb) in (("dve", 0, qr, t1, t2), ("pool", 1024, kr, t3, t4)):
                if src0 == 0 and not full:
                    continue
                xv = p.ap[:, src0:src0 + 1024].rearrange("p (a h f) -> p a h f", a=8, h=2)
                dv = dstt.ap.rearrange("p (a h f) -> p a h f", a=8, h=2)
                x1, x2 = xv[:, :, 0, :], xv[:, :, 1, :]
                tav = ta.ap.rearrange("p (a f) -> p a f", a=8)
                tbv = tb.ap.rearrange("p (a f) -> p a f", a=8)
                self.tt(eng, tav, x1, cv, ALU.mult, [p, c_], [ta])
                self.tt(eng, tbv, x2, sv, ALU.mult, [p, s_], [tb])
                self.tt(eng, dv[:, :, 0, :], tav, tbv, ALU.subtract, [ta, tb], [dstt])
                self.tt(eng, tav, x1, sv, ALU.mult, [p, s_], [ta])
                self.tt(eng, tbv, x2, cv, ALU.mult, [p, c_], [tb])
                self.tt(eng, dv[:, :, 1, :], tav, tbv, ALU.add, [ta, tb], [dstt])
            vv = vb[g % 2]
            self.cp("pool", vv.ap[:, :], p.ap[:, 2048:4096], [p], [vv])
            self.dma("sync", vsc[g], vv.ap[:, :], [vv], [self.db("vsc", g)], "vb%d" % (g % 2))
            if g < NTP:
                rr = rb[g % 2]
                self.act(rr.ap[:, :], p.ap[:, 4096:6144], AF.Silu, [p], [rr])
                self.dma("sync", sr[g], rr.ap[:, :], [rr], [self.db("sr", g)], "rb%d" % (g % 2))
            for d in range(2):
                if d == 0 and not full:
                    continue
                gl = glT[d]
                self.tr(ptot.ap[0:16, 0:128], p.ap[:, 6144 + 16 * d:6160 + 16 * d], self.ident(), [p, self.consts], [ptot])
                self.cp("dve", gl.ap[0:16, :], ptot.ap[0:16, 0:128], [ptot], [gl])
                for hh in range(2):
                    self.mm(psz[hh].ap[:, :], gl.ap[0:17, :], wg.ap[0:17, d * 1024 + hh * 512:d * 1024 + hh * 512 + 512], True, True, [gl, wg], [psz[hh]])
                for hh in range(2):
                    self.act(ex.ap[:, hh * 512:(hh + 1) * 512], psz[hh].ap[:, :], AF.Exp, [psz[hh]], [ex], scale=-1.0)
                self.act(sp.ap[:, :], ex.ap[:, :], AF.Ln, [ex], [sp], bias=1.0)
                cL = (C_LA, C_LB)[d]
                cR = (C_RA, C_RB)[d]
                for hh in range(2):
                    if full:
                        self.mm(pcum[hh].ap[:, :], self.consts.ap[:, cL:cL + 128], sp.ap[:, hh * 512:(hh + 1) * 512], True, True, [self.consts, sp], [pcum[hh]])
                    self.mm(prem[hh].ap[:, :], self.consts.ap[:, cR:cR + 128], sp.ap[:, hh * 512:(hh + 1) * 512], True, True, [self.consts, sp], [prem[hh]])
                for j in range(8):
                    self.mm(ptot.ap[:, 256 + j:257 + j], sp.ap[:, j * 128:(j + 1) * 128], self.consts.ap[:, C_NEG:C_NEG + 1], True, True, [sp, self.consts], [ptot])
                do = (d * NT0 + g) * 8
                self.act(self.decay.ap[:, do:do + 8], ptot.ap[:, 256:264], AF.Exp, [ptot], [self.decay])
                for hh in range(2):
                    sl = slice(hh * 512, (hh + 1) * 512)
                    if full:
                        self.act(E[0].ap[:, sl], pcum[hh].ap[:, :], AF.Exp, [pcum[hh]], [E[0]], bias=lnsc)
                        self.act(E[1].ap[:, sl], pcum[hh].ap[:, :], AF.Exp, [pcum[hh]], [E[1]], scale=-1.0)
                    self.act(E[2].ap[:, sl], prem[hh].ap[:, :], AF.Exp, [prem[hh]], [E[2]])
                qd_, ki_, ke_ = ob[d]
                if full:
                    self.tt("dve", qd_.ap[:, :], qr.ap[:, :], E[0].ap[:, :], ALU.mult, [qr, E[0]], [qd_])
                    self.tt("dve", ki_.ap[:, :], kr.ap[:, :], E[1].ap[:, :], ALU.mult, [kr, E[1]], [ki_])
                self.tt("pool", ke_.ap[:, :], kr.ap[:, :], E[2].ap[:, :], ALU.mult, [kr, E[2]], [ke_])
                self.dma("sync", ke[d, g], ke_.ap[:, :], [ke_], [self.db("ke%d" % d, g)], "ke%d" % d)
                for (srcb, dstT, dr, nm) in (((qd_, tT[d][0], qdT, "qdT"), (ki_, tT[d][1], kiT, "kiT")) if full else ()):
                    for j in range(8):
                        self.tr(ptr_b[:, j * 128:(j + 1) * 128], srcb.ap[:, j * 128:(j + 1) * 128], self.identb.ap[:, :], [srcb, self.identb], [ptr], inc=(j == 7))
                    self.cp("dve", dstT.ap[:, :], ptr_b[:, :], [ptr], [dstT])
                    self.dma("sync", dr[d, g], dstT.ap[:, :], [dstT], [self.db("%s%d" % (nm, d), g)], "%s%d" % (nm, d))
        S.barrier()
        return arena_keep

    def phase_gla_scan(self, keep):
        S, A = self.S, self.A
        qdT, kiT, ke, vsc, sr = (self.dram[n] for n in ("qdT", "kiT", "ke", "vsc", "sr"))
        ng_in = self.din("gla_norm_g", [1, 512])
        oA = self.dscr("oA", [NTP, 128, 2048])
        og = self.dscr("og", [NTP, 128, 2048], BF16)
        A.off = keep
        rowbuf = A.f32(2048)
        ngb = A.f32(512)
        kEb = [A.bf16(1024) for _ in range(2)]
        Vb = [A.bf16(2048) for _ in range(2)]
        QTb = [A.bf16(1024) for _ in range(2)]
        KTb = [A.bf16(1024) for _ in range(2)]
        OAb = [A.f32(2048) for _ in range(2)]
        SRb = [A.bf16(2048) for _ in range(2)]
        st = [A.f32(1024) for _ in range(4)]
        stb = [A.bf16(1024) for _ in range(4)]
        oasb = [A.f32(2048) for _ in range(2)]
        ogsb = [A.bf16(2048) for _ in range(2)]
        osum = [A.f32(512) for _ in range(2)]
        tmp = [A.f32(512) for _ in range(2)]
        junk = A.f32(512)
        attm = [A.bf16(128) for _ in range(2)]
        self.bcast_row(ngb, ngb.ap, ng_in[0:1, :], 512, None, rowbuf, "rowbuf")
        n = 0
        steps = [(0, g) for g in range(NTP)] + [(1, g) for g in [1, 0] + list(range(NT0 - 1, NCTX_T - 1, -1))]

        def sld(idx):
            d_, g_ = steps[idx]
            b_ = idx % 2
            self.dma("sync", kEb[b_].ap[:, :], ke[d_, g_], [self.db("ke%d" % d_, g_)], [kEb[b_]], "kE%d" % b_)
            self.dma("sync", Vb[b_].ap[:, :], vsc[g_], [self.db("vsc", g_)], [Vb[b_]], "V%d" % b_)
            if g_ < NTP:
                self.dma("sync", QTb[b_].ap[:, :], qdT[d_, g_], [self.db("qdT%d" % d_, g_)], [QTb[b_]], "QT%d" % b_)
                self.dma("sync", KTb[b_].ap[:, :], kiT[d_, g_], [self.db("kiT%d" % d_, g_)], [KTb[b_]], "KT%d" % b_)
                if d_ == 1:
                    self.dma("sync", OAb[b_].ap[:, :], oA[g_], [self.db("oA", g_)], [OAb[b_]], "OA%d" % b_)
                    self.dma("sync", SRb[b_].ap[:, :], sr[g_], [self.db("sr", g_)], [SRb[b_]], "SR%d" % b_)

        sld(0)
        for d in range(2):
            order = list(range(NTP)) if d == 0 else [1, 0] + list(range(NT0 - 1, NCTX_T - 1, -1))
            cM = (C_MA, C_MB)[d]
            for h in range(4):
                self.ms("pool", st[h].ap[:, :], 0.0, [st[h]])
                self.ms("pool", stb[h].ap[:, :], 0.0, [stb[h]])
            for g in order:
                out = g < NTP
                b = n % 2
                n += 1
                kE, V, QT, KT, OA, SR = kEb[b], Vb[b], QTb[b], KTb[b], OAb[b], SRb[b]
                assert steps[n - 1] == (d, g)
                if n < len(steps):
                    sld(n)
                oas, ogs = oasb[b], ogsb[b]
                for h in range(4):
                    sel = h % 2
                    patt, po, pu = self.ps[4 * sel], self.ps[4 * sel + 1], [self.ps[4 * sel + 2], self.ps[4 * sel + 3]]
                    hc = slice(h * 512, (h + 1) * 512)
                    if out:
                        for dc in range(2):
                            cc = slice((2 * h + dc) * 128, (2 * h + dc + 1) * 128)
                            self.mm(patt.ap[:, 0:128], KT.ap[:, cc], QT.ap[:, cc], dc == 0, dc == 1, [KT, QT], [patt])
                        am = attm[h % 2]
                        self.tt("dve", am.ap[:, :], patt.ap[:, 0:128], self.consts.ap[:, cM:cM + 128], ALU.mult, [patt, self.consts], [am])
                        self.mm(po.ap[:, :], am.ap[:, :], V.ap[:, hc], True, False, [am, V], [po])
                        for dc in range(2):
                            cc = slice((2 * h + dc) * 128, (2 * h + dc + 1) * 128)
                            self.mm(po.ap[:, :], QT.ap[:, cc], stb[h].ap[:, dc * 512:(dc + 1) * 512], False, dc == 1, [QT, stb[h]], [po])
                        if d == 0:
                            self.cp("act", oas.ap[:, hc], po.ap[:, :], [po], [oas])
                        else:
                            os_, tp_ = osum[h % 2], tmp[h % 2]
                            sq = self.small.ap[:, h:h + 1]
                            rs = self.small.ap[:, 8 + h:9 + h]
                            self.tt("dve", os_.ap[:, :], po.ap[:, :], OA.ap[:, hc], ALU.add, [po, OA], [os_])
                            self.act(junk.ap[:, :], os_.ap[:, :], AF.Square, [os_], [junk, self.small], accum=sq)
                            self.act(rs, sq, AF.Ln, [self.small], [self.small], scale=1.0 / 512, bias=EPS)
                            self.act(rs, rs, AF.Exp, [self.small], [self.small], scale=-0.5)
                            self.stt("dve", tp_.ap[:, :], os_.ap[:, :], rs, ngb.ap[:, :], ALU.mult, ALU.mult, [os_, self.small, ngb], [tp_])
                            self.tt("pool", ogs.ap[:, hc], tp_.ap[:, :], SR.ap[:, hc], ALU.mult, [tp_, SR], [ogs])
                    for dc in range(2):
                        cc = slice((2 * h + dc) * 128, (2 * h + dc + 1) * 128)
                        self.mm(pu[dc].ap[:, :], kE.ap[:, cc], V.ap[:, hc], True, True, [kE, V], [pu[dc]])
                        do = (d * NT0 + g) * 8 + 2 * h + dc
                        ss = slice(dc * 512, (dc + 1) * 512)
                        self.stt("dve", st[h].ap[:, ss], st[h].ap[:, ss], self.decay.ap[:, do:do + 1], pu[dc].ap[:, :], ALU.mult, ALU.add,
                                 [st[h], self.decay, pu[dc]], [st[h]])
                        self.cp("act", stb[h].ap[:, ss], st[h].ap[:, ss], [st[h]], [stb[h]])
                if out:
                    if d == 0:
                        self.dma("sync", oA[g], oas.ap[:, :], [oas], [self.db("oA", g)], "oas%d" % b)
                    else:
                        self.dma("sync", og[g], ogs.ap[:, :], [ogs], [self.db("og", g)], "ogs%d" % b)
        S.barrier()

    def phase_outproj(self, l, w_out, tiles, xsrc, lng_in, lnb_in, wr_in, br_in):
        S, A = self.S, self.A
        og = self.dram["og"]
        modrow = self.dram["modrow"]
        if "h1" not in self.dram:
            self.dscr("h1", [NTP, 128, 2048])
            self.dscr("ftm", [NTP, 128, 2048], BF16)
        h1d, ftm = self.dram["h1"], self.dram["ftm"]
        A.reset()
        wo = A.bf16(16 * 2048)
        rowbuf = A.f32(2048)
        gtb = [A.f32(2048) for _ in range(2)]
        lngb = A.f32(2048)
        lnbb = A.f32(2048)
        ogb = [A.bf16(2048)] * 2
        ogT = [A.bf16(2048) for _ in range(2)]
        xt = [A.f32(2048)] * 2
        tt_ = A.f32(2048)
        h1 = [A.f32(2048) for _ in range(2)]
        fTb = [A.bf16(2048)] * 2
        fT32 = A.f32(2048)
        opb = [A.f32(2048) for _ in range(2)]
        shb = [A.f32(2048) for _ in range(2)]
        wr = A.f32(16 * 20)
        br = A.f32(20)
        lg = A.f32(64)
        stats = A.f32(4 * 6)
        wov = wo.ap.rearrange("p (k n) -> p k n", k=16)
        for cb in range(4):
            self.dma("pool", wov[:, :, cb * 512:(cb + 1) * 512], w_out.rearrange("(k p) n -> p k n", p=128)[:, :, cb * 512:(cb + 1) * 512], [], [wo], "wo")
        self.dma("sync", wr.ap.rearrange("p (k n) -> p k n", k=16), wr_in.rearrange("(k p) n -> p k n", p=128), [], [wr], "wr")
        self.dma("sync", br.ap[0:1, :], br_in, [], [br], "br")
        kinds = sorted(set(1 if g < NCTX_T else 0 for g in tiles))
        for kind in kinds:
            self.bcast_row(gtb[kind], gtb[kind].ap, modrow[l, kind:kind + 1, 2 * D:3 * D], D, self.db("modrow", l), rowbuf, "rowbuf")
        self.bcast_row(lngb, lngb.ap, lng_in, D, None, rowbuf, "rowbuf")
        self.bcast_row(lnbb, lnbb.ap, lnb_in, D, None, rowbuf, "rowbuf")
        for kind in kinds:
            self.bcast_row(opb[kind], opb[kind].ap, modrow[l, kind:kind + 1, 4 * D:5 * D], D, self.db("modrow", l), rowbuf, "rowbuf")
            self.ts("dve", opb[kind].ap[:, :], opb[kind].ap[:, :], 1.0, None, ALU.add, None, [opb[kind]], [opb[kind]])
            self.bcast_row(shb[kind], shb[kind].ap, modrow[l, kind:kind + 1, 3 * D:4 * D], D, self.db("modrow", l), rowbuf, "rowbuf")
        sm = self.small.ap
        cut = int(os.environ.get("OP_CUT", 99))

        def old_(i_):
            g_ = tiles[i_]
            b_ = i_ % 2
            self.dma("sync", ogb[b_].ap[:, :], og[g_], [self.db("og", g_)], [ogb[b_]], "ogb%d" % b_)
            src, dep = xsrc(g_)
            self.dma("sync", xt[b_].ap[:, :], src, [dep] if dep is not None else [], [xt[b_]], "xt%d" % b_)

        for i, g in enumerate(tiles):
            if cut < 1:
                break
            kind = 1 if g < NCTX_T else 0
            b = i % 2
            o_, oT, x_, h_, f_ = ogb[b], ogT[b], xt[b], h1[b], fTb[b]
            if i == 0:
                old_(0)
            for half in range(2):
                pb = self.ps[4 + half]
                pbb = pb.ap.bitcast(BF16)
                for j in range(8):
                    k = half * 8 + j
                    self.tr(pbb[:, j * 128:(j + 1) * 128], o_.ap[:, k * 128:(k + 1) * 128], self.identb.ap[:, :], [o_, self.identb], [pb], inc=(j == 7))
                self.cp("act" if half else "dve", oT.ap[:, half * 1024:(half + 1) * 1024], pbb[:, :], [pb], [oT])
            for cb in range(4):
                pb = self.ps[cb]
                for k in range(NCH):
                    self.mm(pb.ap[:, :], oT.ap[:, k * 128:(k + 1) * 128], wov[:, k, cb * 512:(cb + 1) * 512], k == 0, k == NCH - 1, [oT, wo], [pb])
                cs_ = slice(cb * 512, (cb + 1) * 512)
                self.tt("dve", tt_.ap[:, cs_], pb.ap[:, :], gtb[kind].ap[:, cs_], ALU.mult, [pb, gtb[kind]], [tt_])
            self.stt("dve", tt_.ap[:, :], x_.ap[:, :], ALPHA, tt_.ap[:, :], ALU.mult, ALU.add, [x_, tt_], [tt_])
            if i + 1 < len(tiles):
                old_(i + 1)
            if cut < 2:
                continue
            self.layer_norm(tt_, h_, lngb, lnbb, stats)
            self.dma("sync", h1d[g], h_.ap[:, :], [h_], [self.db("h1", g)], "h1_%d" % b)
            self.tt("pool", tt_.ap[:, :], h_.ap[:, :], opb[kind].ap[:, :], ALU.mult, [h_, opb[kind]], [tt_])
            self.tt("pool", f_.ap.rearrange("t (k m) -> t m k", k=16), tt_.ap.rearrange("t (m k) -> t m k", k=16),
                    shb[kind].ap.rearrange("t (m k) -> t m k", k=16), ALU.add, [tt_, shb[kind]], [f_])
            self.dma("sync", ftm[i], f_.ap[:, :], [f_], [self.db("ftm", i)], "ftm")
            if cut < 3:
                continue
            for q in range(4):
                pb = self.ps[4 + q]
                for j in range(4):
                    k = q * 4 + j
                    self.tr(pb.ap[:, j * 128:(j + 1) * 128], h_.ap[:, k * 128:(k + 1) * 128], self.ident(), [h_, self.consts], [pb], inc=(j == 3))
                for j in range(4):
                    k = q * 4 + j
                    ks = slice(k * 128, (k + 1) * 128)
                    self.act(fT32.ap[:, ks], pb.ap[:, j * 128:(j + 1) * 128], AF.Identity, [pb, self.onepT, self.modT], [fT32],
                             scale=self.ocol(l, kind, 1, k), bias=self.mcol(l, kind, 3, k))
            if cut < 4:
                continue
            pl = self.ps[3]
            wrv = wr.ap.rearrange("p (k n) -> p k n", k=16)
            for k in range(NCH):
                self.mm(pl.ap[:, 0:20], fT32.ap[:, k * 128:(k + 1) * 128], wrv[:, k, :], k == 0, False, [fT32, wr], [pl])
            self.mm(pl.ap[:, 0:20], self.consts.ap[0:1, C_ONE:C_ONE + 128], br.ap[0:1, :], False, True, [self.consts, br], [pl])
            self.cp("dve", lg.ap[:, 0:20], pl.ap[:, 0:20], [pl], [lg])
            if not os.environ.get('NO_ROUTING'):
                self.routing(lg, i)
        S.barrier()

    def layer_norm(self, xin, hout, lngb, lnbb, stats):
        sm = self.small
        for c in range(4):
            self.S.op("dve", lambda e, c=c: e.bn_stats(stats.ap[:, c * 6:(c + 1) * 6], xin.ap[:, c * 512:(c + 1) * 512]), reads=[xin], writes=[stats])
        self.S.op("dve", lambda e: e.bn_aggr(sm.ap[:, 16:18], stats.ap[:, 0:24]), reads=[stats], writes=[sm])
        self.act(sm.ap[:, 18:19], sm.ap[:, 17:18], AF.Ln, [sm], [sm], bias=EPS)
        self.act(sm.ap[:, 18:19], sm.ap[:, 18:19], AF.Exp, [sm], [sm], scale=-0.5)
        self.ts("dve", xin.ap[:, :], xin.ap[:, :], sm.ap[:, 16:17], sm.ap[:, 18:19], ALU.subtract, ALU.mult, [xin, sm], [xin])
        self.tt("pool", xin.ap[:, :], xin.ap[:, :], lngb.ap[:, :], ALU.mult, [xin, lngb], [xin])
        self.tt("dve", hout.ap[:, :], xin.ap[:, :], lnbb.ap[:, :], ALU.add, [xin, lnbb], [hout])

    def routing(self, lg, i):
        sm = self.small
        L = lg.ap
        c = lambda a, b=None: sm.ap[:, a:(b if b is not None else a + 1)]
        R, W = [lg, sm], [sm]
        self.S.op("dve", lambda e: e.reduce_max(out=c(20), in_=L[:, 0:4], axis=AX.X), reads=R, writes=W)
        self.ts("dve", c(21), c(20), -1.0, None, ALU.mult, None, R, W)
        self.act(c(24, 28), L[:, 0:4], AF.Exp, R, W, bias=c(21), accum=c(22))
        self.S.op("dve", lambda e: e.reciprocal(c(23), c(22)), reads=R, writes=W)
        self.ts("dve", c(28, 32), L[:, 0:4], c(20), None, ALU.is_equal, None, R, W)
        self.ts("dve", c(32, 36), L[:, 4:8], c(28), None, ALU.mult, None, R, W)
        for g in range(1, 4):
            self.stt("dve", c(32, 36), L[:, 4 + 4 * g:8 + 4 * g], c(28 + g), c(32, 36), ALU.mult, ALU.add, R, W)
        self.S.op("dve", lambda e: e.reduce_max(out=c(36), in_=c(32, 36), axis=AX.X), reads=R, writes=W)
        self.ts("dve", c(37), c(36), -1.0, None, ALU.mult, None, R, W)
        self.act(c(40, 44), c(32, 36), AF.Exp, R, W, bias=c(37))
        self.ts("dve", c(44, 48), c(32, 36), c(36), None, ALU.is_equal, None, R, W)
        self.stt("dve", c(48, 52), c(44, 48), -1e30, c(32, 36), ALU.mult, ALU.add, R, W)
        self.S.op("dve", lambda e: e.reduce_max(out=c(38), in_=c(48, 52), axis=AX.X), reads=R, writes=W)
        self.ts("dve", c(52, 56), c(48, 52), c(38), None, ALU.is_equal, None, R, W)
        self.tt("dve", c(52, 56), c(52, 56), c(44, 48), ALU.add, R, W)
        self.tt("dve", c(40, 44), c(40, 44), c(52, 56), ALU.mult, R, W)
        self.S.op("dve", lambda e: e.reduce_sum(out=c(39), in_=c(40, 44), axis=AX.X), reads=R, writes=W)
        self.S.op("dve", lambda e: e.reciprocal(c(39), c(39)), reads=R, writes=W)
        self.tt("dve", c(39), c(39), c(23), ALU.mult, R, W)
        self.ts("dve", c(40, 44), c(40, 44), c(39), None, ALU.mult, None, R, W)
        for g in range(4):
            o = i * 16 + g * 4
            self.ts("dve", self.wt_all.ap[:, o:o + 4], c(40, 44), c(28 + g), None, ALU.mult, None, R, [self.wt_all])
            self.ts("dve", self.A_all.ap[:, o:o + 4], c(52, 56), c(28 + g), None, ALU.mult, None, R, [self.A_all])

    def phase_moe(self, l, ntiles, w1, w3, w2):
        S, A = self.S, self.A
        fTd = self.dram["fT"]
        A.reset()
        acc = A.bf16(ntiles * 2048)
        self.moe_acc = acc
        keep = A.off
        W1 = [A.bf16(16 * 256) for _ in range(2)]
        W3 = [A.bf16(16 * 256) for _ in range(2)]
        W2 = [A.bf16(2 * 2048) for _ in range(2)]
        fg = [A.bf16(16 * 512) for _ in range(2)]
        hT = [A.bf16(512) for _ in range(4)]
        su = [A.bf16(512) for _ in range(2)]
        ngrp = ntiles // 4
        half = ntiles * 1024
        self.ms("pool", acc.ap[:, 0:half], 0.0, [acc])
        self.ms("dve", acc.ap[:, half:2 * half], 0.0, [acc])
        n = 0
        ny = 0
        nh = 0
        for e_ in range(16):
            for qt in range(4):
                u = e_ * 4 + qt
                ub = u % 2
                w1v = W1[ub].ap.rearrange("p (k n) -> p k n", k=16)
                w3v = W3[ub].ap.rearrange("p (k n) -> p k n", k=16)
                w2v = W2[ub].ap.rearrange("p (c n) -> p c n", c=2)
                qs = slice(qt * 256, (qt + 1) * 256)
                self.dma("pool", w1v, w1[l, e_].rearrange("(k p) n -> p k n", p=128)[:, :, qs], [], [W1[ub]], "W1_%d" % ub)
                self.dma("pool", w3v, w3[l, e_].rearrange("(k p) n -> p k n", p=128)[:, :, qs], [], [W3[ub]], "W3_%d" % ub)
                self.dma("pool", w2v, w2[l, e_, qs, :].rearrange("(c p) n -> p c n", p=128), [], [W2[ub]], "W2_%d" % ub)
                for gi in range(ngrp):
                    f = fg[n % 2]
                    fv = f.ap.rearrange("p (k n) -> p k n", k=16)
                    self.dma("sync", fv, fTd[gi], [self.db("fT", gi)], [f], "fg%d" % (n % 2))
                    n += 1
                    hs = []
                    for dcq in range(2):
                        pu, pv = self.ps[2 * (nh % 2)], self.ps[2 * (nh % 2) + 1]
                        h_ = hT[nh % 4]
                        s_ = su[nh % 2]
                        nh += 1
                        ds = slice(dcq * 128, (dcq + 1) * 128)
                        for k in range(NCH):
                            self.mm(pu.ap[:, :], w1v[:, k, ds], fv[:, k, :], k == 0, k == NCH - 1, [W1[ub], f], [pu])
                        for k in range(NCH):
                            self.mm(pv.ap[:, :], w3v[:, k, ds], fv[:, k, :], k == 0, k == NCH - 1, [W3[ub], f], [pv])
                        self.act(s_.ap[:, :], pu.ap[:, :], AF.Silu, [pu], [s_])
                        self.tt("dve", h_.ap[:, :], pv.ap[:, :], s_.ap[:, :], ALU.mult, [pv, s_], [h_])
                        hs.append(h_)
                    for t in range(4):
                        i = gi * 4 + t
                        wcol = self.wt_all.ap[:, i * 16 + e_:i * 16 + e_ + 1]
                        for cb in range(4):
                            py = self.ps[4 + ny % 4]
                            ny += 1
                            for dcq in range(2):
                                self.mm(py.ap[:, :], hs[dcq].ap[:, t * 128:(t + 1) * 128], w2v[:, dcq, cb * 512:(cb + 1) * 512], dcq == 0, dcq == 1, [hs[dcq], W2[ub]], [py])
                            asl = acc.ap[:, i * 2048 + cb * 512:i * 2048 + (cb + 1) * 512]
                            self.stt("dve", asl, py.ap[:, :], wcol, asl, ALU.mult, ALU.add, [py, self.wt_all, acc], [acc])
        S.barrier()
        return keep

    def phase_sort(self, ntiles, l=0):
        S, A = self.S, self.A
        ftm = self.dram["ftm"]
        NB = 2 * ntiles * 128 // MOE_B + 16
        Xs = self.dram["Xs"]
        A.reset()
        cnt = A.f32(16)
        nb = A.f32(16)
        tmp = A.f32(16)
        pst = A.f32(16)
        pen = A.f32(16)
        be = A.f32(32)
        dst = A.f32(16)
        dA = A.f32(16)
        eq = A.f32(16)
        col = A.f32(8)
        fb = [A.bf16(2048) for _ in range(2)]
        Aall, wall = self.A_all, self.wt_all
        pc = self.ps[0]
        for i in range(ntiles):
            self.mm(pc.ap[:, 0:16], self.consts.ap[:, C_ONE:C_ONE + 128], Aall.ap[:, i * 16:(i + 1) * 16], i == 0, i == ntiles - 1, [self.consts, Aall], [pc])
        self.cp("dve", cnt.ap[:, :], pc.ap[:, 0:16], [pc], [cnt])
        nmax = ntiles * 128 // MOE_B
        self.ts("dve", nb.ap[:, :], cnt.ap[:, :], 0.0, None, ALU.is_gt, None, [cnt], [nb])
        for m in range(1, nmax):
            self.ts("dve", tmp.ap[:, :], cnt.ap[:, :], float(m * MOE_B), None, ALU.is_gt, None, [cnt], [tmp])
            self.tt("dve", nb.ap[:, :], nb.ap[:, :], tmp.ap[:, :], ALU.add, [nb, tmp], [nb])
        self.ts("dve", nb.ap[:, :], nb.ap[:, :], float(MOE_B), None, ALU.mult, None, [nb], [nb])
        self.ms("dve", pst.ap[:, 0:1], 0.0, [pst])
        for e_ in range(1, 16):
            self.tt("dve", pst.ap[:, e_:e_ + 1], pst.ap[:, e_ - 1:e_], nb.ap[:, e_ - 1:e_], ALU.add, [pst, nb], [pst])
        self.tt("dve", pen.ap[:, :], pst.ap[:, :], nb.ap[:, :], ALU.add, [pst, nb], [pen])
        for b in range(NB):
            self.ts("dve", tmp.ap[:, :], pen.ap[:, :], float(b * MOE_B), None, ALU.is_le, None, [pen], [tmp])
            self.S.op("dve", lambda e, b=b: e.reduce_sum(out=be.ap[:, b:b + 1], in_=tmp.ap[:, :], axis=AX.X), reads=[tmp], writes=[be])
        self.ts("dve", be.ap[:, 0:NB], be.ap[:, 0:NB], 15.0, None, ALU.min, None, [be], [be])
        for b in range(NB):
            for q in range(4):
                self.stt("dve", col.ap[:, 0:1], be.ap[:, b:b + 1], 512.0, self.consts.ap[:, C_P4 + q:C_P4 + q + 1], ALU.mult, ALU.add, [be, self.consts], [col])
                if l:
                    self.ts("dve", col.ap[:, 0:1], col.ap[:, 0:1], float(l * 8192), None, ALU.add, None, [col], [col])
                self.cp("dve", self.widx.ap[:, b * 12 + q:b * 12 + q + 1], col.ap[:, 0:1], [col], [self.widx])
            for c_ in range(8):
                self.stt("dve", col.ap[:, 0:1], be.ap[:, b:b + 1], 1024.0, self.consts.ap[:, C_PC + c_:C_PC + c_ + 1], ALU.mult, ALU.add, [be, self.consts], [col])
                if l:
                    self.ts("dve", col.ap[:, 0:1], col.ap[:, 0:1], float(l * 16384), None, ALU.add, None, [col], [col])
                self.cp("dve", self.widx.ap[:, b * 12 + 4 + c_:b * 12 + 5 + c_], col.ap[:, 0:1], [col], [self.widx])
        for i in range(ntiles):
            pr = self.ps[1 + i % 2]
            for j in range(i):
                self.mm(pr.ap[:, 0:16], self.consts.ap[:, C_ONE:C_ONE + 128], Aall.ap[:, j * 16:(j + 1) * 16], j == 0, False, [self.consts, Aall], [pr])
            self.mm(pr.ap[:, 0:16], self.consts.ap[:, C_SLT:C_SLT + 128], Aall.ap[:, i * 16:(i + 1) * 16], i == 0, True, [self.consts, Aall], [pr])
            Ai = Aall.ap[:, i * 16:(i + 1) * 16]
            wi = wall.ap[:, i * 16:(i + 1) * 16]
            self.tt("dve", dst.ap[:, :], pr.ap[:, 0:16], pst.ap[:, :], ALU.add, [pr, pst], [dst])
            self.tt("dve", dA.ap[:, :], dst.ap[:, :], Ai, ALU.mult, [dst, Aall], [dA])
            self.S.op("dve", lambda e: e.reduce_sum(out=col.ap[:, 1:2], in_=dA.ap[:, :], axis=AX.X), reads=[dA], writes=[col])
            self.S.op("dve", lambda e: e.reduce_max(out=col.ap[:, 2:3], in_=dA.ap[:, :], axis=AX.X), reads=[dA], writes=[col])
            self.tt("dve", col.ap[:, 3:4], col.ap[:, 1:2], col.ap[:, 2:3], ALU.subtract, [col], [col])
            self.cp("dve", self.didx.ap[:, 2 * i:2 * i + 1], col.ap[:, 3:4], [col], [self.didx])
            self.cp("dve", self.didx.ap[:, 2 * i + 1:2 * i + 2], col.ap[:, 2:3], [col], [self.didx])
            self.ts("dve", col.ap[:, 5:7], col.ap[:, 2:4], float(26 * MOE_B), None, ALU.add, None, [col], [col])
            self.cp("dve", self.didx2.ap[:, 2 * i:2 * i + 1], col.ap[:, 6:7], [col], [self.didx2])
            self.cp("dve", self.didx2.ap[:, 2 * i + 1:2 * i + 2], col.ap[:, 5:6], [col], [self.didx2])
            self.ts("dve", eq.ap[:, :], dA.ap[:, :], col.ap[:, 2:3], None, ALU.is_equal, None, [dA, col], [eq])
            self.tt("dve", eq.ap[:, :], eq.ap[:, :], wi, ALU.mult, [eq, wall], [eq])
            self.S.op("dve", lambda e, i=i: e.reduce_sum(out=self.dw.ap[:, 2 * i + 1:2 * i + 2], in_=eq.ap[:, :], axis=AX.X), reads=[eq], writes=[self.dw])
            self.S.op("dve", lambda e, wi=wi: e.reduce_sum(out=col.ap[:, 4:5], in_=wi, axis=AX.X), reads=[wall], writes=[col])
            self.tt("dve", self.dw.ap[:, 2 * i:2 * i + 1], col.ap[:, 4:5], self.dw.ap[:, 2 * i + 1:2 * i + 2], ALU.subtract, [col, self.dw], [self.dw])
            f = fb[i % 2]
            self.dma("sync", f.ap[:, :], ftm[i], [self.db("ftm", i)], [f], "fb%d" % (i % 2))
            for w_ in range(2):
                self.S.op("pool", lambda e, f=f, i=i, w_=w_: e.indirect_dma_start(
                    out=self.dram["big2d"], out_offset=bass.IndirectOffsetOnAxis(ap=self.didx.ap[:, 2 * i + w_:2 * i + w_ + 1], axis=0),
                    in_=f.ap[:, :], in_offset=None), reads=[f, self.didx], writes=[self.db("Xs", 0)], dma="xsc%d" % (i % 2))
        S.barrier()
        return NB

    def phase_moe_sorted(self, l, NB, w1, w3, w2):
        S, A = self.S, self.A
        Xs = self.dram["Xs"]
        Yd = self.dram["Yd"]
        w1v = w1.rearrange("l e (p q r) c -> (l e p q) (r c)", q=4, r=4)
        w3v = w3.rearrange("l e (p q r) c -> (l e p q) (r c)", q=4, r=4)
        w2v = w2.rearrange("l e r c -> (l e r) c")
        A.reset()
        W1 = A.bf16(16 * 1024)
        W3 = A.bf16(16 * 1024)
        W2 = A.bf16(8 * 2048)
        xsb = [A.bf16(2048) for _ in range(4)]
        XT = A.bf16(16 * MOE_B)
        hT = A.bf16(8 * MOE_B)
        su = [A.bf16(MOE_B) for _ in range(2)]
        yst = [A.bf16(2048) for _ in range(2)]
        W1v_ = W1.ap.rearrange("p (k c) -> p k c", k=16)
        W3v_ = W3.ap.rearrange("p (k c) -> p k c", k=16)
        W2v_ = W2.ap.rearrange("p (c n) -> p c n", c=8)
        XTv = XT.ap.rearrange("p (k s) -> p k s", k=16)
        hTv = hT.ap.rearrange("p (c s) -> p c s", c=8)
        ny = 0
        nh = 0
        mcut = int(os.environ.get("MOE_CUT", 99))
        if mcut < 5:
            NB = 1
        for b in range(NB):
            for (Wt, Wv_, src, nm) in ((W1, W1v_, w1v, "gw1"), (W3, W3v_, w3v, "gw3")):
                for q in range(4):
                    self.S.op("pool", lambda e, Wt=Wt, src=src, q=q, b=b: e.indirect_dma_start(
                        out=Wt.ap[:, q * 4096:(q + 1) * 4096], out_offset=None, in_=src,
                        in_offset=bass.IndirectOffsetOnAxis(ap=self.widx.ap[:, b * 12 + q:b * 12 + q + 1], axis=0)),
                        reads=[self.widx], writes=[Wt], dma=nm)
            if mcut < 1:
                continue
            for t in range(4):
                self.dma("sync", xsb[t].ap[:, :], Xs[b * MOE_B + t * 128:b * MOE_B + (t + 1) * 128, :], [self.db("Xs", 0)], [xsb[t]], "xsb%d" % t)
            for t in range(4):
                xv = xsb[t].ap.rearrange("p (k m) -> p k m", k=16)
                for half in range(2):
                    pb = self.ps[6 + half]
                    pbb = pb.ap.bitcast(BF16)
                    for j in range(8):
                        k = half * 8 + j
                        self.tr(pbb[:, j * 128:(j + 1) * 128], xv[:, k, :], self.identb.ap[:, :], [xsb[t], self.identb], [pb], inc=(j == 7))
                    self.cp("act" if half else "dve", XTv[:, half * 8:(half + 1) * 8, t * 128:(t + 1) * 128], pbb.rearrange("p (j s) -> p j s", j=8), [pb], [XT])
            if mcut < 2:
                continue
            for c_ in range(8):
                pu, pv = self.ps[2 * (nh % 2)], self.ps[2 * (nh % 2) + 1]
                s_ = su[nh % 2]
                nh += 1
                ds_ = slice(c_ * 128, (c_ + 1) * 128)
                for k in range(NCH):
                    self.mm(pu.ap[:, :], W1v_[:, k, ds_], XTv[:, k, :], k == 0, k == NCH - 1, [W1, XT], [pu])
                for k in range(NCH):
                    self.mm(pv.ap[:, :], W3v_[:, k, ds_], XTv[:, k, :], k == 0, k == NCH - 1, [W3, XT], [pv])
                self.act(s_.ap[:, :], pu.ap[:, :], AF.Silu, [pu], [s_])
                self.tt("dve", hTv[:, c_, :], pv.ap[:, :], s_.ap[:, :], ALU.mult, [pv, s_], [hT])
            if mcut < 3:
                continue
            for c_ in range(8):
                self.S.op("pool", lambda e, c_=c_, b=b: e.indirect_dma_start(
                    out=W2.ap[:, c_ * 2048:(c_ + 1) * 2048], out_offset=None, in_=w2v,
                    in_offset=bass.IndirectOffsetOnAxis(ap=self.widx.ap[:, b * 12 + 4 + c_:b * 12 + 5 + c_], axis=0)),
                    reads=[self.widx], writes=[W2], dma="gw2")
            if mcut < 4:
                continue
            for t in range(4):
                ys = yst[t % 2]
                for cb in range(4):
                    py = self.ps[4 + ny % 2]
                    ny += 1
                    for c_ in range(8):
                        self.mm(py.ap[:, :], hTv[:, c_, t * 128:(t + 1) * 128], W2v_[:, c_, cb * 512:(cb + 1) * 512], c_ == 0, c_ == 7, [hT, W2], [py])
                    self.cp("act" if cb % 2 else "dve", ys.ap[:, cb * 512:(cb + 1) * 512], py.ap[:, :], [py], [ys])
                self.dma("sync", Yd[b * MOE_B + t * 128:b * MOE_B + (t + 1) * 128, :], ys.ap[:, :], [ys], [self.db("Yd", 0)], "yst%d" % (t % 2))
        S.barrier()

    def phase_ln2(self, l, tiles, keep, lng_in, lnb_in, dst_fn, sorted_=False):
        S, A = self.S, self.A
        h1d = self.dram["h1"]
        modrow = self.dram["modrow"]
        acc = None if sorted_ else self.moe_acc
        A.off = keep
        if sorted_:
            Yd = self.dram["Yd"]
            ylo = [A.bf16(2048) for _ in range(2)]
            yhi = [A.bf16(2048) for _ in range(2)]
        rowbuf = A.f32(2048)
        gtb = [A.f32(2048) for _ in range(2)]
        lngb = A.f32(2048)
        lnbb = A.f32(2048)
        ht = [A.f32(2048) for _ in range(2)]
        tt_ = [A.f32(2048) for _ in range(2)]
        ho = [A.f32(2048) for _ in range(2)]
        stats = A.f32(24)
        kinds = sorted(set(1 if g < NCTX_T else 0 for g in tiles))
        for kind in kinds:
            self.bcast_row(gtb[kind], gtb[kind].ap, modrow[l, kind:kind + 1, 5 * D:6 * D], D, self.db("modrow", l), rowbuf, "rowbuf")
        self.bcast_row(lngb, lngb.ap, lng_in, D, None, rowbuf, "rowbuf")
        self.bcast_row(lnbb, lnbb.ap, lnb_in, D, None, rowbuf, "rowbuf")
        def lld(i_):
            g_ = tiles[i_]
            b_ = i_ % 2
            self.dma("sync", ht[b_].ap[:, :], h1d[g_], [self.db("h1", g_)], [ht[b_]], "ht%d" % b_)
            if sorted_:
                for (yb, w_) in ((ylo[b_], 0), (yhi[b_], 1)):
                    self.S.op("pool", lambda e, yb=yb, i_=i_, w_=w_: e.indirect_dma_start(
                        out=yb.ap[:, :], out_offset=None, in_=self.dram["big2d"],
                        in_offset=bass.IndirectOffsetOnAxis(ap=self.didx2.ap[:, 2 * i_ + w_:2 * i_ + w_ + 1], axis=0)),
                        reads=[self.didx2, self.db("Yd", 0)], writes=[yb], dma="yg%d_%d" % (w_, b_))

        lld(0)
        for i, g in enumerate(tiles):
            kind = 1 if g < NCTX_T else 0
            b = i % 2
            if i + 1 < len(tiles):
                lld(i + 1)
            if sorted_:
                pass
                self.ts("dve", tt_[b].ap[:, :], ylo[b].ap[:, :], self.dw.ap[:, 2 * i:2 * i + 1], None, ALU.mult, None, [ylo[b], self.dw], [tt_[b]])
                self.stt("dve", tt_[b].ap[:, :], yhi[b].ap[:, :], self.dw.ap[:, 2 * i + 1:2 * i + 2], tt_[b].ap[:, :], ALU.mult, ALU.add, [yhi[b], self.dw, tt_[b]], [tt_[b]])
                self.tt("dve", tt_[b].ap[:, :], tt_[b].ap[:, :], gtb[kind].ap[:, :], ALU.mult, [tt_[b], gtb[kind]], [tt_[b]])
            else:
                self.tt("dve", tt_[b].ap[:, :], acc.ap[:, i * 2048:(i + 1) * 2048], gtb[kind].ap[:, :], ALU.mult, [acc, gtb[kind]], [tt_[b]])
            self.stt("dve", tt_[b].ap[:, :], ht[b].ap[:, :], ALPHA, tt_[b].ap[:, :], ALU.mult, ALU.add, [ht[b], tt_[b]], [tt_[b]])
            self.layer_norm(tt_[b], ho[b], lngb, lnbb, stats)
            dst, dbuf = dst_fn(g)
            self.dma("sync", dst, ho[b].ap[:, :], [ho[b]], [dbuf], "ho%d" % b)
        S.barrier()

    def phase_nat_inproj(self):
        S, A = self.S, self.A
        h2 = self.dram["h2"]
        w_in = self.din("nat_w_in", [D, 3 * D])
        qT = self.dscr("nqT", [NTP, 128, 16, 128], BF16)
        kT = self.dscr("nkT", [16, 128, NTP * 128], BF16)
        va = self.dscr("nva", [NTP, 128, 16, 132], BF16)
        A.reset()
        aT = A.bf16(NTP * 2048)
        xs = [A.f32(D) for _ in range(2)]
        wsl = [A.bf16(16 * 512) for _ in range(2)]
        stg = [A.bf16(512) for _ in range(4)]
        vst = [A.bf16(4 * 132) for _ in range(2)]
        tiles = list(range(NTP))
        self.build_aT(aT, tiles, lambda g: (h2[g], self.db("h2", g)), 1, 0, 0, xs, gm=True)
        for v_ in vst:
            vv = v_.ap.rearrange("p (h c) -> p h c", h=4)
            self.ms("pool", v_.ap[:, :], 0.0, [v_])
            self.ms("pool", vv[:, :, 128:129], 1.0, [v_])
        wv = w_in.rearrange("(k p) n -> p k n", p=128)
        aTv = aT.ap.rearrange("p (g k n) -> p g k n", g=NTP // 4, k=16)
        n = 0
        nv = 0
        qscale = 128.0 ** -0.5
        for sl_ in range(12):
            w = wsl[sl_ % 2]
            wap = w.ap.rearrange("p (k n) -> p k n", k=16)
            self.dma("pool", wap, wv[:, :, sl_ * 512:(sl_ + 1) * 512], [], [w], "nwsl%d" % (sl_ % 2))
            if sl_ < 8:
                for h4 in range(4):
                    hh = (sl_ % 4) * 4 + h4
                    for grp in range(NTP // 4):
                        pb = self.ps[n % 4]
                        st = stg[n % 4]
                        n += 1
                        for k in range(NCH):
                            self.mm(pb.ap[:, :], wap[:, k, h4 * 128:(h4 + 1) * 128], aTv[:, grp, k, :], k == 0, k == NCH - 1, [w, aT], [pb])
                        if sl_ < 4:
                            self.act(st.ap[:, :], pb.ap[:, :], AF.Copy, [pb], [st], scale=qscale)
                            self.dma("sync", qT[grp * 4:grp * 4 + 4, :, hh, :].rearrange("t p q -> p t q"), st.ap.rearrange("p (t q) -> p t q", t=4),
                                     [st], [self.db("nqT", grp)], "nstg%d" % ((n - 1) % 4))
                        else:
                            self.cp("dve", st.ap[:, :], pb.ap[:, :], [pb], [st])
                            self.dma("sync", kT[hh, :, grp * 512:(grp + 1) * 512], st.ap[:, :], [st], [self.db("nkT", 0)], "nstg%d" % ((n - 1) % 4))
            else:
                cb = sl_ - 8
                for i in range(NTP):
                    pb = self.ps[4 + n % 4]
                    n += 1
                    v_ = vst[nv % 2]
                    for k in range(NCH):
                        self.mm(pb.ap[:, :], aTv[:, i // 4, k, (i % 4) * 128:(i % 4 + 1) * 128], wap[:, k, :], k == 0, k == NCH - 1, [w, aT], [pb])
                    vv = v_.ap.rearrange("p (h c) -> p h c", h=4)
                    self.cp("dve" if nv % 2 else "act", vv[:, :, 0:128], pb.ap.rearrange("p (h c) -> p h c", h=4), [pb], [v_])
                    self.dma("sync", va[i, :, cb * 4:(cb + 1) * 4, :], vv, [v_], [self.db("nva", i)], "nvst%d" % (nv % 2))
                    nv += 1
        S.barrier()

    def phase_nat_attn(self):
        S, A = self.S, self.A
        qT, kT, va, og = self.dram["nqT"], self.dram["nkT"], self.dram["nva"], self.dram["og"]
        bias_in = self.din("nat_bias", [3, 16, 128, 5 * 128])
        A.reset()
        qb = [A.bf16(16 * 128) for _ in range(2)]
        kb_ = [A.bf16(16 * 896) for _ in range(2)]
        vb = [A.bf16(7 * 16 * 132) for _ in range(2)]
        ball = A.f32(16 * 640)
        sb_ = [A.f32(640) for _ in range(2)]
        pT = [A.bf16(896) for _ in range(2)]
        ogt = [A.bf16(2048) for _ in range(2)]
        bav = ball.ap.rearrange("p (h n) -> p h n", h=16)

        def loads(j):
            g = j + NCTX_T
            kbr = min(max(2 * j - 4, 0), 26)
            b = j % 2
            q_, k_, v_ = qb[b], kb_[b], vb[b]
            kv = k_.ap.rearrange("p (h t) -> p h t", h=16)
            vv = v_.ap.rearrange("p (c h e) -> p c h e", c=7, h=16)
            self.dma("sync", q_.ap.rearrange("p (h q) -> p h q", h=16), qT[g], [self.db("nqT", g // 4)], [q_], "nq%d" % b)
            t0 = 256 + 64 * kbr
            self.dma("sync", kv[:, :, 0:640], kT[:, :, t0:t0 + 640].rearrange("h p t -> p h t"), [self.db("nkT", 0)], [k_], "nk%d" % b)
            self.dma("sync", kv[:, :, 640:896], kT[:, :, 0:256].rearrange("h p t -> p h t"), [self.db("nkT", 0)], [k_], "nk%d" % b)
            for c in range(7):
                gt = (NCTX_T + kbr // 2 + c) if c < 5 else (c - 5)
                self.dma("sync", vv[:, c, :, :], va[gt], [self.db("nva", gt)], [v_], "nv%d" % b)

        n = 0
        loads(0)
        last_cls = -1
        for j in range(NOWN_T):
            g = j + NCTX_T
            cls = min(j, 2)
            b = j % 2
            q_, k_, v_ = qb[b], kb_[b], vb[b]
            kv = k_.ap.rearrange("p (h t) -> p h t", h=16)
            vv = v_.ap.rearrange("p (c h e) -> p c h e", c=7, h=16)
            if cls != last_cls:
                for hq in range(4):
                    self.dma("sync", bav[:, hq * 4:(hq + 1) * 4, :], bias_in[cls, hq * 4:(hq + 1) * 4].rearrange("h p n -> p h n"), [], [ball], "nball")
                last_cls = cls
            if j + 1 < NOWN_T:
                loads(j + 1)
            o_ = ogt[b]
            for h in range(16):
                hb = n % 2
                n += 1
                pS0, pS1, pO = self.ps[3 * hb], self.ps[3 * hb + 1], self.ps[3 * hb + 2]
                sc_, p_ = sb_[hb], pT[hb]
                for c in range(7):
                    pd = pS0 if c < 4 else pS1
                    self.mm(pd.ap[:, (c % 4) * 128:(c % 4 + 1) * 128], kv[:, h, c * 128:(c + 1) * 128], q_.ap[:, h * 128:(h + 1) * 128], True, True, [k_, q_], [pd])
                self.tt("dve", sc_.ap[:, 0:512], pS0.ap[:, :], bav[:, h, 0:512], ALU.add, [pS0, ball], [sc_])
                self.tt("dve", sc_.ap[:, 512:640], pS1.ap[:, 0:128], bav[:, h, 512:640], ALU.add, [pS1, ball], [sc_])
                self.act(p_.ap[:, 640:896], pS1.ap[:, 128:384], AF.Exp, [pS1], [p_])
                self.act(p_.ap[:, 0:640], sc_.ap[:, :], AF.Exp, [sc_], [p_])
                for c in range(7):
                    self.mm(pO.ap[:, 0:129], p_.ap[:, c * 128:(c + 1) * 128], vv[:, c, h, 0:129], c == 0, c == 6, [p_, v_], [pO])
                rc = self.small.ap[:, 60 + hb:61 + hb]
                self.S.op("dve", lambda e, rc=rc, pO=pO: e.reciprocal(rc, pO.ap[:, 128:129]), reads=[pO], writes=[self.small])
                self.ts("dve", o_.ap[:, h * 128:(h + 1) * 128], pO.ap[:, 0:128], rc, None, ALU.mult, None, [pO, self.small], [o_])
            self.dma("sync", og[g], o_.ap[:, :], [o_], [self.db("og", g)], "nog%d" % b)
        S.barrier()

    def finish(self, out_waits):
        for t in out_waits:
            tok = t.buf.w
            if tok is not None:
                k, v = tok
                if self.S.waited["sync"].get(k, 0) < v:
                    self.S.streams["sync"].append(("w", k, v))
                    self.S.waited["sync"][k] = v
        self.S.barrier()
        self.S.emit(self.nc)
        return self.nc


LASTP = None
SORTED = True


def build(debug=None, stop=None):
    global LASTP
    P = Prog(debug)
    LASTP = P
    P.setup()
    P.phase_mod()
    if stop == "mod":
        return P.finish([])
    P.phase_gla_inproj()
    if stop == "inproj":
        return P.finish([])
    keep = P.phase_gla_prep()
    if stop == "prep":
        return P.finish([])
    P.phase_gla_scan(keep)
    if stop == "scan":
        return P.finish([])
    x, ctx = P.dram["x"], P.dram["ctx"]
    ln_g = P.din("ln_g", [2, 2, D])
    ln_b = P.din("ln_b", [2, 2, D])
    wr = P.din("moe_wr", [2, D, 20])
    br = P.din("moe_br", [2, 20])
    w1 = P.din("moe_w1", [2, 16, D, 1024])
    w3 = P.din("moe_w3", [2, 16, D, 1024])
    w2 = P.din("moe_w2", [2, 16, 1024, D])
    gla_w_out = P.din("gla_w_out", [D, D])

    def xsrc0(g):
        if g < NCTX_T:
            return ctx[g * 128:(g + 1) * 128, :], None
        return x[(g - NCTX_T) * 128:(g - NCTX_T + 1) * 128, :], None

    tiles0 = list(range(NTP))
    P.phase_outproj(0, gla_w_out, tiles0, xsrc0, ln_g[0, 0:1, :], ln_b[0, 0:1, :], wr[0], br[0:1, :])
    if stop == "outproj0":
        return P.finish([])
    h2 = P.dscr("h2", [NTP, 128, D])
    if SORTED:
        NB = P.phase_sort(NTP)
        if stop == "sort0":
            return P.finish([])
        P.phase_moe_sorted(0, NB, w1, w3, w2)
        if stop == "moe0":
            return P.finish([])
        P.A.reset()
        P.phase_ln2(0, tiles0, 0, ln_g[0, 1:2, :], ln_b[0, 1:2, :], lambda g: (h2[g], P.db("h2", g)), sorted_=True)
    else:
        keep = P.phase_moe(0, NTP, w1, w3, w2)
        P.phase_ln2(0, tiles0, keep, ln_g[0, 1:2, :], ln_b[0, 1:2, :], lambda g: (h2[g], P.db("h2", g)))
    if stop == "l0":
        return P.finish([])
    P.phase_nat_inproj()
    P.phase_nat_attn()
    if stop == "nat":
        return P.finish([])
    nat_w_out = P.din("nat_w_out", [D, D])
    own = list(range(NCTX_T, NCTX_T + NOWN_T))
    P.phase_outproj(1, nat_w_out, own, lambda g: (h2[g], P.db("h2", g)), ln_g[1, 0:1, :], ln_b[1, 0:1, :], wr[1], br[1:2, :])
    if stop == "outproj1":
        return P.finish([])
    out = P.dout("out", [NOWN_T * 128, D])
    outb = Tn(None)
    if SORTED:
        NB = P.phase_sort(NOWN_T, 1)
        P.phase_moe_sorted(1, NB, w1, w3, w2)
        P.A.reset()
        P.phase_ln2(1, own, 0, ln_g[1, 1:2, :], ln_b[1, 1:2, :], lambda g: (out[(g - NCTX_T) * 128:(g - NCTX_T + 1) * 128, :], outb), sorted_=True)
    else:
        keep = P.phase_moe(1, NOWN_T, w1, w3, w2)
        P.phase_ln2(1, own, keep, ln_g[1, 1:2, :], ln_b[1, 1:2, :], lambda g: (out[(g - NCTX_T) * 128:(g - NCTX_T + 1) * 128, :], outb))
    return P.finish([outb])


def rope_tables(flip):
    t = np.arange(NLAT_T * 128)
    tt = (NLAT_T * 128 - 1 - t) if flip else t
    freqs = (10000.0 ** (-np.arange(64, dtype=np.float32) / 64)).astype(np.float32)
    ang_r = (tt // 64).astype(np.float32)[:, None] * freqs
    ang_c = (tt % 64).astype(np.float32)[:, None] * freqs
    cos = np.ones((NT0 * 128, 8, 64), np.float32)
    sin = np.zeros((NT0 * 128, 8, 64), np.float32)
    for h in range(4):
        cos[256:, 2 * h] = np.cos(ang_r)
        sin[256:, 2 * h] = np.sin(ang_r)
        cos[256:, 2 * h + 1] = np.cos(ang_c)
        sin[256:, 2 * h + 1] = np.sin(ang_c)
    return cos.reshape(NT0, 128, 512), sin.reshape(NT0, 128, 512)


def nat_bias_table(rpb, flip):
    out = np.empty((3, 16, 128, 5, 128), np.float32)
    for cls in range(3):
        j = cls
        kb = min(max(2 * j - 4, 0), 26)
        q = np.arange(128)
        qr_p, qc_p = 2 * j + q // 64, q % 64
        kk = np.arange(640)
        kr_p, kc_p = kb + kk // 64, kk % 64
        if flip:
            qr, qc, kr, kc = 63 - qr_p, 63 - qc_p, 63 - kr_p, 63 - kc_p
        else:
            qr, qc, kr, kc = qr_p, qc_p, kr_p, kc_p
        rs = np.clip(qr - 4, 0, 56)[None, :]
        cs = np.clip(qc - 8, 0, 48)[None, :]
        KR, KC = kr[:, None], kc[:, None]
        valid = (KR >= rs) & (KR < rs + 8) & (KC >= cs) & (KC < cs + 16)
        assert (valid.sum(0) == 128).all()
        dr = np.clip(KR - qr[None, :] + 7, 0, 14)
        dc = np.clip(KC - qc[None, :] + 15, 0, 30)
        for h in range(16):
            bias = np.where(valid, rpb[h][dr, dc], np.float32(NEG)).astype(np.float32)
            out[cls, h] = bias.reshape(5, 128, 128).transpose(1, 0, 2)
    return out.reshape(3, 16, 128, 640)


def host_inputs(inp):
    consts = make_consts()
    ropes = [rope_tables(False), rope_tables(True)]
    wr = np.ascontiguousarray(np.concatenate([inp["moe_w_group"], inp["moe_w_expert"]], axis=2))
    br = np.ascontiguousarray(np.concatenate([inp["moe_b_group"], inp["moe_b_expert"]], axis=1))
    nbias = [nat_bias_table(inp["nat_rpb"][0], False), nat_bias_table(inp["nat_rpb"][0], True)]
    maps = []
    for core in range(8):
        b, hf = core // 2, core % 2
        x = inp["x"][b]
        ctx = inp["ctx"][b]
        w_in = inp["gla_w_in"][0]
        wg = np.concatenate([inp["gla_w_gate"][0], inp["gla_b_gate"][0][:, None, :]], axis=1)
        if hf:
            x = x[::-1]
            ctx = ctx[::-1]
            w_in = np.concatenate([w_in[:, :6144], w_in[:, 6160:6176], w_in[:, 6144:6160]], axis=1)
            wg = wg[::-1]
        m = {
            "consts_in": consts,
            "cc": np.ascontiguousarray(np.stack([inp["c"][b], inp["c_ctx"]], 0)),
            "ada_w": inp["ada_w"], "ada_b": inp["ada_b"],
            "x": np.ascontiguousarray(x), "ctx": np.ascontiguousarray(ctx),
            "gla_w_in": np.ascontiguousarray(w_in),
            "gla_wg": np.ascontiguousarray(wg),
            "rope_cos": ropes[hf][0], "rope_sin": ropes[hf][1],
            "gla_norm_g": np.ascontiguousarray(inp["gla_norm_g"][0][None, :]),
            "gla_w_out": inp["gla_w_out"][0],
            "ln_g": inp["ln_g"], "ln_b": inp["ln_b"],
            "moe_wr": wr, "moe_br": br,
            "moe_w1": inp["moe_w1"], "moe_w3": inp["moe_w3"], "moe_w2": inp["moe_w2"],
            "nat_w_in": inp["nat_w_in"][0], "nat_w_out": inp["nat_w_out"][0], "nat_bias": nbias[hf],
        }
        maps.append(m)
    return maps


_NC = None


def kernel(**inputs):
    global _NC
    inp = {k: np.asarray(v) for k, v in inputs.items()}
    if _NC is None:
        _NC = build()
    nc = _NC
    maps = host_inputs(inp)
    names = set(LASTP.dram.keys())
    maps = [{k: np.ascontiguousarray(v, dtype=np.float32) for k, v in m.items() if k in names} for m in maps]
    res = run_bass_kernel_spmd(nc, maps, core_ids=list(range(8)))
    out = np.empty((4, 4096, D), np.float32)
    for core in range(8):
        b, hf = core // 2, core % 2
        o = res.results[core]["out"]
        if hf:
            out[b, 2048:] = o[::-1]
        else:
            out[b, :2048] = o
    return out
```
